# Optimizing a Trainium2 kernel written in Bass

```python
import math
import jax, jax.numpy as jnp
from jax import lax
import numpy as np

D_MODEL = 1024
BATCH = 8
SEQ = 4096
DEPTH = 2

SB_HEADS = 8
SB_HEAD_DIM = 64
SB_WIDTH = SB_HEADS * SB_HEAD_DIM
BLOCK_Q = 128
LRU_WIDTH = 512
LRU_BLOCKS = 8
LRU_BLOCK = LRU_WIDTH // LRU_BLOCKS
LRU_CONV = 4
LRU_C = 8.0
SC_WIDTH = 512
SC_CONV = 3
N_BRANCH = 3
N_GROUPS = 4
EXP_PER_GROUP = 4
N_EXPERTS = N_GROUPS * EXP_PER_GROUP
TOP_K = 2
D_EXPERT = 512
ALPHA = (2 * DEPTH) ** 0.25
BETA = (8 * DEPTH) ** -0.25
LN_EPS = 1e-5

PROJ_SPLITS = [SB_WIDTH] * 3 + [LRU_WIDTH] * 2 + [SC_WIDTH] * 3 + [D_MODEL] * N_BRANCH
PROJ_COLS = sum(PROJ_SPLITS)
SPLIT_POINTS = [sum(PROJ_SPLITS[:i + 1]) for i in range(len(PROJ_SPLITS) - 1)]

kernel_name = "hybrid_sb_rglru_shortconv_hmoe_deepnorm"


def layer_norm(x, g, b):
    xf = x.astype(jnp.float32)
    mu = jnp.mean(xf, axis=-1, keepdims=True)
    var = jnp.mean(jnp.square(xf - mu), axis=-1, keepdims=True)
    y = (xf - mu) * lax.rsqrt(var + LN_EPS) * g.astype(jnp.float32) + b.astype(jnp.float32)
    return y.astype(x.dtype)


def causal_dwconv(u, w):
    K = w.shape[0]
    S = u.shape[1]
    up = jnp.pad(u, ((0, 0), (K - 1, 0), (0, 0)))
    return sum(w[k] * up[:, k:k + S] for k in range(K))


def stick_breaking_attention(q, k, v):
    B, S, H, Dh = q.shape
    nb = S // BLOCK_Q
    qf = q.astype(jnp.float32) * (Dh ** -0.5)
    kf = k.astype(jnp.float32)
    vf = v.astype(jnp.float32)
    q_blocks = qf.reshape(B, nb, BLOCK_Q, H, Dh).transpose(1, 0, 3, 2, 4)
    starts = jnp.arange(nb, dtype=jnp.int32) * BLOCK_Q
    key_pos = jnp.arange(S, dtype=jnp.int32)

    def one_block(args):
        qb, start = args
        z = jnp.einsum('bhqd,bkhd->bhqk', qb, kf)
        q_pos = start + jnp.arange(BLOCK_Q, dtype=jnp.int32)
        before = key_pos[None, :] < q_pos[:, None]
        log_keep = jnp.where(before, jax.nn.log_sigmoid(-z), 0.0)
        log_pass = lax.cumsum(log_keep, axis=3, reverse=True) - log_keep
        w = jnp.where(before, jnp.exp(jax.nn.log_sigmoid(z) + log_pass), 0.0)
        return jnp.einsum('bhqk,bkhd->bqhd', w, vf)

    o = lax.map(one_block, (q_blocks, starts))
    return o.transpose(1, 0, 2, 3, 4).reshape(B, S, H * Dh).astype(q.dtype)


def rg_lru(u, w_a, b_a, w_x, b_x, lam):
    B, S, W = u.shape
    uf = u.astype(jnp.float32)
    ub = uf.reshape(B, S, LRU_BLOCKS, LRU_BLOCK)
    r = jax.nn.sigmoid(jnp.einsum('bshi,hij->bshj', ub, w_a.astype(jnp.float32)).reshape(B, S, W)
                       + b_a.astype(jnp.float32))
    i = jax.nn.sigmoid(jnp.einsum('bshi,hij->bshj', ub, w_x.astype(jnp.float32)).reshape(B, S, W)
                       + b_x.astype(jnp.float32))
    log_a = -LRU_C * r * jax.nn.softplus(-lam.astype(jnp.float32))
    a = jnp.exp(log_a)
    drive = jnp.sqrt(-jnp.expm1(2.0 * log_a)) * (i * uf)

    def combine(left, right):
        a1, b1 = left
        a2, b2 = right
        return a1 * a2, a2 * b1 + b2

    _, h = lax.associative_scan(combine, (a, drive), axis=1)
    return h.astype(u.dtype)


def hybrid_mixer(x, w_in, gate_b, lru_conv_w, lru_conv_b, lru_wa, lru_ba, lru_wx, lru_bx,
                 lru_lambda, sc_conv_w, w_branch_sb, w_branch_lru, w_branch_sc, w_out):
    B, S, _ = x.shape
    zc = jnp.einsum('bsd,de->bse', x, w_in)
    (q, k, v, lru_in, lru_gate, sc_b, sc_c, sc_h,
     g_sb, g_lru, g_sc) = jnp.split(zc, SPLIT_POINTS, axis=-1)
    hs = (B, S, SB_HEADS, SB_HEAD_DIM)
    y_sb = stick_breaking_attention(q.reshape(hs), k.reshape(hs), v.reshape(hs))
    u = causal_dwconv(lru_in, lru_conv_w) + lru_conv_b
    y_lru = jax.nn.gelu(lru_gate) * rg_lru(u, lru_wa, lru_ba, lru_wx, lru_bx, lru_lambda)
    y_sc = sc_b * causal_dwconv(sc_c * sc_h, sc_conv_w)
    merged = (jax.nn.sigmoid(g_sb + gate_b[0]) * jnp.einsum('bsw,wd->bsd', y_sb, w_branch_sb)
              + jax.nn.sigmoid(g_lru + gate_b[1]) * jnp.einsum('bsw,wd->bsd', y_lru, w_branch_lru)
              + jax.nn.sigmoid(g_sc + gate_b[2]) * jnp.einsum('bsw,wd->bsd', y_sc, w_branch_sc))
    return jnp.einsum('bsd,de->bse', merged, w_out)


def hierarchical_moe(x, w_group, group_bias, w_expert_router, expert_bias, w_gate, w_up, w_down):
    B, S, D = x.shape
    xt = x.reshape(-1, D)
    N = xt.shape[0]
    g_logits = jnp.einsum('nd,dg->ng', xt, w_group).astype(jnp.float32)
    g_prob = jax.nn.softmax(g_logits, axis=-1)
    g_sel = jnp.argmax(g_logits + group_bias.astype(jnp.float32), axis=-1)
    e_logits = jnp.einsum('nd,de->ne', xt, w_expert_router).astype(jnp.float32)
    e_logits = e_logits.reshape(N, N_GROUPS, EXP_PER_GROUP)
    e_in_group = jnp.take_along_axis(e_logits, g_sel[:, None, None], axis=1)[:, 0]
    e_bias = expert_bias.astype(jnp.float32).reshape(N_GROUPS, EXP_PER_GROUP)[g_sel]
    _, top_idx = lax.top_k(e_in_group + e_bias, TOP_K)
    top_w = jax.nn.softmax(jnp.take_along_axis(e_in_group, top_idx, axis=1), axis=-1)
    top_w = top_w * jnp.take_along_axis(g_prob, g_sel[:, None], axis=1)
    expert_id = g_sel[:, None] * EXP_PER_GROUP + top_idx
    combine = jnp.sum(jax.nn.one_hot(expert_id, N_EXPERTS, dtype=jnp.float32) * top_w[..., None],
                      axis=1).astype(x.dtype)
    out = jnp.zeros((N, D), x.dtype)
    for grp in range(N_GROUPS):
        sl = slice(grp * EXP_PER_GROUP, (grp + 1) * EXP_PER_GROUP)
        h = (jax.nn.silu(jnp.einsum('nd,edf->nef', xt, w_gate[sl]))
             * jnp.einsum('nd,edf->nef', xt, w_up[sl]))
        out = out + jnp.einsum('nef,efd->nd', h * combine[:, sl, None], w_down[sl])
    return out.reshape(B, S, D)


def setup_inputs(seed: int = 0) -> dict:
    key = jax.random.key(seed)
    ks = iter(jax.random.split(key, 40))
    f32 = jnp.float32

    def nrm(shape, scale):
        return jax.random.normal(next(ks), shape, f32) * scale

    u = jax.random.uniform(next(ks), (DEPTH, LRU_WIDTH), f32, 0.9, 0.999)
    a0 = u ** (1.0 / LRU_C)
    lru_lambda = jnp.log(a0) - jnp.log1p(-a0)
    return {
        "x": nrm((BATCH, SEQ, D_MODEL), 1.0),
        "ln_in_g": 1.0 + nrm((D_MODEL,), 0.02),
        "ln_in_b": nrm((D_MODEL,), 0.02),
        "w_in": nrm((DEPTH, D_MODEL, PROJ_COLS), D_MODEL ** -0.5),
        "gate_b": nrm((DEPTH, N_BRANCH, D_MODEL), 0.1),
        "lru_conv_w": nrm((DEPTH, LRU_CONV, LRU_WIDTH), LRU_CONV ** -0.5),
        "lru_conv_b": nrm((DEPTH, LRU_WIDTH), 0.02),
        "lru_wa": nrm((DEPTH, LRU_BLOCKS, LRU_BLOCK, LRU_BLOCK), LRU_BLOCK ** -0.5),
        "lru_ba": nrm((DEPTH, LRU_WIDTH), 0.1),
        "lru_wx": nrm((DEPTH, LRU_BLOCKS, LRU_BLOCK, LRU_BLOCK), LRU_BLOCK ** -0.5),
        "lru_bx": nrm((DEPTH, LRU_WIDTH), 0.1),
        "lru_lambda": lru_lambda,
        "sc_conv_w": nrm((DEPTH, SC_CONV, SC_WIDTH), SC_CONV ** -0.5),
        "w_branch_sb": nrm((DEPTH, SB_WIDTH, D_MODEL), BETA * SB_WIDTH ** -0.5),
        "w_branch_lru": nrm((DEPTH, LRU_WIDTH, D_MODEL), BETA * LRU_WIDTH ** -0.5),
        "w_branch_sc": nrm((DEPTH, SC_WIDTH, D_MODEL), BETA * SC_WIDTH ** -0.5),
        "w_out": nrm((DEPTH, D_MODEL, D_MODEL), BETA * D_MODEL ** -0.5),
        "ln1_g": 1.0 + nrm((DEPTH, D_MODEL), 0.02),
        "ln1_b": nrm((DEPTH, D_MODEL), 0.02),
        "w_group": nrm((DEPTH, D_MODEL, N_GROUPS), D_MODEL ** -0.5),
        "group_bias": nrm((DEPTH, N_GROUPS), 0.01),
        "w_expert_router": nrm((DEPTH, D_MODEL, N_EXPERTS), D_MODEL ** -0.5),
        "expert_bias": nrm((DEPTH, N_EXPERTS), 0.01),
        "w_gate": nrm((DEPTH, N_EXPERTS, D_MODEL, D_EXPERT), D_MODEL ** -0.5),
        "w_up": nrm((DEPTH, N_EXPERTS, D_MODEL, D_EXPERT), D_MODEL ** -0.5),
        "w_down": nrm((DEPTH, N_EXPERTS, D_EXPERT, D_MODEL), BETA * D_EXPERT ** -0.5),
        "ln2_g": 1.0 + nrm((DEPTH, D_MODEL), 0.02),
        "ln2_b": nrm((DEPTH, D_MODEL), 0.02),
    }


def reference(x, ln_in_g, ln_in_b, w_in, gate_b, lru_conv_w, lru_conv_b, lru_wa, lru_ba,
              lru_wx, lru_bx, lru_lambda, sc_conv_w, w_branch_sb, w_branch_lru, w_branch_sc,
              w_out, ln1_g, ln1_b, w_group, group_bias, w_expert_router, expert_bias,
              w_gate, w_up, w_down, ln2_g, ln2_b):
    h = layer_norm(x, ln_in_g, ln_in_b)
    for l in range(DEPTH):
        mix = hybrid_mixer(h, w_in[l], gate_b[l], lru_conv_w[l], lru_conv_b[l], lru_wa[l],
                           lru_ba[l], lru_wx[l], lru_bx[l], lru_lambda[l], sc_conv_w[l],
                           w_branch_sb[l], w_branch_lru[l], w_branch_sc[l], w_out[l])
        h = layer_norm(ALPHA * h + mix, ln1_g[l], ln1_b[l])
        ffn = hierarchical_moe(h, w_group[l], group_bias[l], w_expert_router[l], expert_bias[l],
                               w_gate[l], w_up[l], w_down[l])
        h = layer_norm(ALPHA * h + ffn, ln2_g[l], ln2_b[l])
    return h
```

```python
import numpy as np
from contextlib import ExitStack
import concourse.bass as bass
import concourse.mybir as mybir
from concourse.bass_utils import run_bass_kernel_spmd

F32 = mybir.dt.float32
BF16 = mybir.dt.bfloat16
AF = mybir.ActivationFunctionType
ALU = mybir.AluOpType
AX = mybir.AxisListType

S = 4096
D = 1024
PC = 7168
NT = 8
TT = 512
DEPTH = 2
ALPHA = float((2 * DEPTH) ** 0.25)
LN_EPS = 1e-5
NV = 100
NCORES = 8


class Tok:
    __slots__ = ("w", "r")

    def __init__(self):
        self.w = None
        self.r = {}


class Eng:
    def __init__(self, ctx, eng, name, is_pe=False):
        self.ctx = ctx
        self.eng = eng
        self.name = name
        self.is_pe = is_pe
        self.sem = ctx.es.enter_context(ctx.nc.semaphore("s_" + name))
        self.count = 0
        self.waited = {}

    def wait(self, ev):
        sem, val, src = ev
        if src is self and self.is_pe:
            return
        if self.waited.get(sem, 0) >= val:
            return
        self.eng.wait_ge(sem, val)
        self.waited[sem] = val

    def signal(self, ins):
        self.count += 1
        ins.then_inc(self.sem, 1)
        return (self.sem, self.count, self)


class Ctx:
    NDMA = 24

    def __init__(self, nc):
        self.nc = nc
        self.es = ExitStack()
        self.pe = Eng(self, nc.tensor, "pe", True)
        self.act = Eng(self, nc.scalar, "act")
        self.dve = Eng(self, nc.vector, "dve")
        self.pool = Eng(self, nc.gpsimd, "pool")
        self.sp = Eng(self, nc.sync, "sp")
        self.engs = [self.pe, self.act, self.dve, self.pool, self.sp]
        self.dsem = [[self.es.enter_context(nc.semaphore("d%d" % i)), 0] for i in range(self.NDMA)]
        self.di = {}
        self.uid = 0

    def _deps(self, E, reads, writes):
        for b in reads:
            if b.w is not None:
                E.wait(b.w)
        for b in writes:
            if b.w is not None:
                E.wait(b.w)
            for ev in b.r.values():
                E.wait(ev)

    def _reg(self, ev, reads, writes):
        for b in reads:
            b.r[ev[0]] = ev
        for b in writes:
            b.w = ev
            b.r = {}

    def op(self, E, fn, reads=(), writes=()):
        self._deps(E, reads, writes)
        ins = fn()
        ev = E.signal(ins)
        self._reg(ev, reads, writes)
        return ev

    def dma(self, Q, out_ap, in_ap, reads=(), writes=()):
        self._deps(Q, reads, writes)
        half = self.NDMA // 2
        base = 0 if Q is self.sp else half
        n = self.di.get(base, 0)
        self.di[base] = n + 1
        slot = self.dsem[base + n % half]
        if slot[1] > 0:
            Q.wait((slot[0], 16 * slot[1], None))
        ins = Q.eng.dma_start(out=out_ap, in_=in_ap)
        slot[1] += 1
        ins.then_inc(slot[0], 16)
        ev = (slot[0], 16 * slot[1], None)
        self._reg(ev, reads, writes)
        return ev

    def mm(self, out_ap, pairs, reads=(), writes=(), start=True, stop=True):
        n = len(pairs)

        def fn():
            ins = None
            for i, (l, r) in enumerate(pairs):
                ins = self.nc.tensor.matmul(out_ap, l, r, start=(start and i == 0), stop=(stop and i == n - 1))
            return ins
        return self.op(self.pe, fn, reads, writes)

    def barrier(self):
        evs = [(E.sem, E.count, E) for E in self.engs if E.count > 0]
        evs += [(s[0], 16 * s[1], None) for s in self.dsem if s[1] > 0]
        for E in self.engs:
            for ev in evs:
                if ev[2] is not E:
                    E.wait(ev)

    def name(self, p):
        self.uid += 1
        return "%s_%d" % (p, self.uid)


class Scope:
    def __init__(self, ctx):
        self.ctx = ctx
        self.es = ExitStack()

    def sb(self, shape, dt, name="t"):
        return self.es.enter_context(self.ctx.nc.sbuf_tensor(self.ctx.name(name), list(shape), dt))

    def ps(self, shape=(128, 512), dt=F32, name="p"):
        return self.es.enter_context(self.ctx.nc.psum_tensor(self.ctx.name(name), list(shape), dt))

    def close(self):
        self.es.close()


def build(nlayers=DEPTH, stop=None, debug=False):
    nc = bass.Bass("TRN2", target_bir_lowering=False)

    def din(name, shape, dt=F32):
        return nc.dram_tensor(name, list(shape), dt, kind="ExternalInput").ap()

    def dscr(name, shape, dt):
        return nc.dram_tensor(name, list(shape), dt, kind=("ExternalOutput" if debug else "Internal")).ap()

    x_d = din("x", [S, D])
    w_in_d = din("w_in", [DEPTH, D, PC])
    wa_d = din("wa_bd", [DEPTH, 4, 128, 128])
    wx_d = din("wx_bd", [DEPTH, 4, 128, 128])
    wbr_d = din("w_branch", [DEPTH, 3, 512, D])
    wout_d = din("w_out", [DEPTH, D, D])
    wr_d = din("w_router", [DEPTH, D, 20])
    wg_d = din("w_gate", [DEPTH, 16, D, 512])
    wu_d = din("w_up", [DEPTH, 16, D, 512])
    wd_d = din("w_down", [DEPTH, 16, 512, D])
    vecs_d = din("vecs", [DEPTH, 128, NV])
    lnin_d = din("lnin", [128, 16])
    rb_d = din("rbias", [DEPTH, 128, 20])
    ident_d = din("ident", [128, 128])
    negtri_d = din("negtri", [128, 128])
    masks_d = din("masks", [128, 8 * 512])
    sel_d = din("sel", [16, 16 * 128])
    out_d = nc.dram_tensor("out", [S, D], F32, kind="ExternalOutput").ap()

    h32_d = dscr("h32", [NT, 128, 8, TT], F32)
    hbf_d = dscr("hbf", [NT, 128, 8, TT], BF16)
    ysb_d = dscr("ysb", [512, S], BF16)
    ylru_d = dscr("ylru", [512, S], BF16)
    ysc_d = dscr("ysc", [512, S], BF16)
    mT_d = dscr("mT", [NT, 128, 8, TT], BF16)

    def fm(ap):
        return ap.rearrange("(c p) t -> p c t", p=128)

    C = Ctx(nc)
    pe, act, dve, pool, sp = C.engs

    G = Scope(C)
    ident = G.sb([128, 128], F32, "ident")
    negtri = G.sb([128, 128], BF16, "negtri")
    ones_bf = G.sb([128, 128], BF16, "ones")
    negones = G.sb([128, 128], BF16, "negones")
    mean_bf = G.sb([128, 128], BF16, "meanm")
    masks = G.sb([128, 8 * 512], BF16, "masks")
    sel = G.sb([16, 16 * 128], BF16, "sel")
    lnin = G.sb([128, 16], F32, "lnin")
    vecs = G.sb([128, DEPTH * NV], F32, "vecs")
    rbias = G.sb([128, DEPTH * 20], F32, "rbias")
    cchan = G.sb([128, DEPTH * 4], F32, "cchan")
    wr_sb = G.sb([128, 8, 20], F32, "wr")
    lneps = G.sb([128, 1], F32, "lneps")
    lg = G.sb([128, 32, 20], F32, "lg")
    t_wr, t_lg = Tok(), Tok()
    k_const = Tok()
    C.dma(sp, ident[:], ident_d[:, :], writes=[k_const])
    C.dma(pool, negtri[:], negtri_d[:, :], writes=[k_const])
    C.dma(pool, masks[:], masks_d[:, :], writes=[k_const])
    C.dma(pool, sel[:], sel_d[:, :], writes=[k_const])
    C.dma(sp, lnin[:], lnin_d[:, :], writes=[k_const])
    for l in range(DEPTH):
        C.dma(sp, vecs[:, l * NV:(l + 1) * NV], vecs_d[l], writes=[k_const])
        C.dma(sp, rbias[:, l * 20:(l + 1) * 20], rb_d[l], writes=[k_const])
    C.op(dve, lambda: nc.vector.memset(ones_bf[:], 1.0), writes=[k_const])
    C.op(dve, lambda: nc.vector.memset(mean_bf[:], 1.0 / D), writes=[k_const])
    C.op(dve, lambda: nc.vector.memset(negones[:], -1.0), writes=[k_const])
    C.op(dve, lambda: nc.vector.memset(lneps[:], LN_EPS), writes=[k_const])
    C.barrier()
    for l in range(DEPTH):
        C.op(act, lambda l=l: nc.scalar.activation(out=cchan[:, l * 4:(l + 1) * 4], in_=vecs[:, l * NV + 84:l * NV + 88],
                                                   func=AF.Exp, scale=-1.0), reads=[k_const], writes=[k_const])
        C.op(act, lambda l=l: nc.scalar.activation(out=cchan[:, l * 4:(l + 1) * 4], in_=cchan[:, l * 4:(l + 1) * 4],
                                                   func=AF.Ln, bias=1.0), reads=[k_const], writes=[k_const])
    C.op(dve, lambda: nc.vector.tensor_scalar(out=cchan[:], in0=cchan[:], scalar1=-8.0, scalar2=None, op0=ALU.mult),
         reads=[k_const], writes=[k_const])
    C.barrier()

    def vcol(l, c0, n=1):
        return vecs[:, l * NV + c0:l * NV + c0 + n]

    def ln_alloc(Sc, nsets, final=False):
        L = {"sets": [], "ps": [], "n": nsets}
        for _ in range(nsets):
            W = {}
            W["xb"] = Sc.sb([128, 8, TT], BF16, "lnxb")
            W["sq"] = Sc.sb([128, 8, TT], BF16, "lnsq")
            W["hb"] = Sc.sb([128, 8, TT], BF16, "lnhb")
            W["mean_sb"] = Sc.sb([128, TT], F32, "lnmean")
            W["msq"] = Sc.sb([128, TT], F32, "lnmsq")
            W["rstd"] = Sc.sb([128, TT], F32, "lnrstd")
            for k in ("t_xb", "t_sq", "t_hb", "t_st"):
                W[k] = Tok()
            W["t_c"] = [Tok() for _ in range(8)]
            L["sets"].append(W)
        for _ in range(2):
            L["ps"].append(dict(mean_ps=Sc.ps(name="lnmps"), ex2_ps=Sc.ps(name="lneps"), t_mps=Tok(), t_eps=Tok()))
        if final:
            L["ot"] = [Sc.sb([128, D], F32, "lnot") for _ in range(2)]
            L["t_ot"] = [Tok(), Tok()]
            L["tp_ps"] = [Sc.ps(name="lntp") for _ in range(2)]
            L["t_tp"] = [Tok(), Tok()]
        return L

    def ln_s1(L, k, xs, xs_tok):
        W = L["sets"][k % L["n"]]
        Pp = L["ps"][k % 2]
        xb, sq = W["xb"], W["sq"]
        C.op(dve, lambda: nc.vector.tensor_copy(out=xb[:], in_=xs[:]), reads=[xs_tok], writes=[W["t_xb"]])
        C.op(act, lambda: nc.scalar.activation(out=sq[:], in_=xs[:], func=AF.Square), reads=[xs_tok], writes=[W["t_sq"]])
        C.mm(Pp["mean_ps"][:], [(mean_bf[:], xb[:, c, :]) for c in range(8)], reads=[W["t_xb"]], writes=[Pp["t_mps"]])
        C.mm(Pp["ex2_ps"][:], [(mean_bf[:], sq[:, c, :]) for c in range(8)], reads=[W["t_sq"]], writes=[Pp["t_eps"]])

    def ln_s2(L, k):
        W = L["sets"][k % L["n"]]
        Pp = L["ps"][k % 2]
        mean_sb, msq, rstd, t_st = W["mean_sb"], W["msq"], W["rstd"], W["t_st"]
        C.op(act, lambda: nc.scalar.activation(out=mean_sb[:], in_=Pp["mean_ps"][:], func=AF.Copy), reads=[Pp["t_mps"]], writes=[t_st])
        C.op(act, lambda: nc.scalar.activation(out=msq[:], in_=Pp["mean_ps"][:], func=AF.Square), reads=[Pp["t_mps"]], writes=[t_st])
        C.op(dve, lambda: nc.vector.tensor_tensor(out=rstd[:], in0=Pp["ex2_ps"][:], in1=msq[:], op=ALU.subtract),
             reads=[Pp["t_eps"], t_st], writes=[t_st])
        C.op(act, lambda: nc.scalar.activation(out=rstd[:], in_=rstd[:], func=AF.Ln, bias=lneps[:, 0:1]), reads=[t_st], writes=[t_st])
        C.op(act, lambda: nc.scalar.activation(out=rstd[:], in_=rstd[:], func=AF.Exp, scale=-0.5), reads=[t_st], writes=[t_st])

    def ln_s3(L, k, xs, xs_tok, gcol, bcol, tile_i, final, router=None):
        W = L["sets"][k % L["n"]]
        hb, t_hb, t_st = W["hb"], W["t_hb"], W["t_st"]
        mean_sb, rstd = W["mean_sb"], W["rstd"]
        for c in range(8):
            E = pool if c in (3, 7) else dve
            ee = E.eng
            tk = W["t_c"][c]
            C.op(E, lambda: ee.tensor_tensor(out=xs[:, c, :], in0=xs[:, c, :], in1=mean_sb[:], op=ALU.subtract),
                 reads=[t_st, xs_tok, W["t_xb"], W["t_sq"]], writes=[tk])
            C.op(E, lambda: ee.tensor_tensor(out=xs[:, c, :], in0=xs[:, c, :], in1=rstd[:], op=ALU.mult),
                 reads=[t_st, tk], writes=[tk])
            if not final:
                C.op(act, lambda: nc.scalar.activation(out=hb[:, c, :], in_=xs[:, c, :], func=AF.Identity,
                                                       scale=gcol[:, c:c + 1], bias=bcol[:, c:c + 1]), reads=[tk], writes=[t_hb])
            C.op(act, lambda: nc.scalar.activation(out=xs[:, c, :], in_=xs[:, c, :], func=AF.Identity,
                                                   scale=gcol[:, c:c + 1], bias=bcol[:, c:c + 1]), reads=[tk], writes=[tk])
        tsl = slice(tile_i * TT, (tile_i + 1) * TT)
        if router is not None:
            wr, t_wr, lpsL, t_lpsL, lg, t_lg, lg_copy = router
            if tile_i > 0:
                lg_copy(tile_i - 1)
            lps, t_lps = lpsL[tile_i % 2], t_lpsL[tile_i % 2]
            for tb in range(4):
                C.mm(lps[:, tb * 32:tb * 32 + 20], [(xs[:, c, tb * 128:(tb + 1) * 128], wr[:, c, :]) for c in range(8)],
                     reads=W["t_c"] + [t_wr], writes=[t_lps])
        if not final:
            C.dma(sp, h32_d[tile_i], xs[:], writes=[xs_tok] + W["t_c"])
            C.dma(sp, hbf_d[tile_i], hb[:], reads=[t_hb])
        else:
            ot, t_ot, tp_ps, t_tp = L["ot"], L["t_ot"], L["tp_ps"], L["t_tp"]
            for tb in range(4):
                for half in range(2):
                    kk = (tb * 2 + half) % 2
                    for cc in range(4):
                        c = half * 4 + cc
                        C.mm(tp_ps[kk][:, cc * 128:(cc + 1) * 128], [(xs[:, c, tb * 128:(tb + 1) * 128], ident[:])],
                             reads=[W["t_c"][c], xs_tok], writes=[t_tp[kk]])
                    C.op(act, lambda: nc.scalar.activation(out=ot[tb % 2][:, half * 512:(half + 1) * 512], in_=tp_ps[kk][:], func=AF.Copy),
                         reads=[t_tp[kk]], writes=[t_ot[tb % 2]])
                r0 = tile_i * TT + tb * 128
                C.dma(sp, out_d[r0:r0 + 128, :], ot[tb % 2][:], reads=[t_ot[tb % 2]])

    def pipeline(n, stages, reverse=False):
        ns = len(stages)
        order = list(enumerate(stages))
        if reverse:
            order = order[::-1]
        for step in range(n + ns - 1):
            for si, st in order:
                if 0 <= step - si < n:
                    st(step - si)

    def phase0():
        Sc = Scope(C)
        L = ln_alloc(Sc, 2)
        xt = [Sc.sb([128, 4, D], F32, "xt") for _ in range(2)]
        t_xt = [Tok(), Tok()]
        xs = [Sc.sb([128, 8, TT], F32, "xs") for _ in range(3)]
        t_xs = [Tok() for _ in range(3)]
        tp = [Sc.ps(name="tp") for _ in range(2)]
        t_tp = [Tok(), Tok()]
        xv = x_d.rearrange("(n tb p) d -> n p tb d", tb=4, p=128)

        def T0(i):
            C.dma(sp, xt[i % 2][:], xv[i], writes=[t_xt[i % 2]])

        def T1(i):
            b = i % 2
            x3 = i % 3
            for c in range(8):
                k = c % 2
                for tb in range(4):
                    C.mm(tp[k][:, tb * 128:(tb + 1) * 128], [(xt[b][:, tb, c * 128:(c + 1) * 128], ident[:])],
                         reads=[t_xt[b]], writes=[t_tp[k]])
                if c % 2 == 0:
                    C.op(act, lambda: nc.scalar.activation(out=xs[x3][:, c, :], in_=tp[k][:], func=AF.Copy),
                         reads=[t_tp[k]], writes=[t_xs[x3]])
                else:
                    C.op(dve, lambda: nc.vector.tensor_copy(out=xs[x3][:, c, :], in_=tp[k][:]), reads=[t_tp[k]], writes=[t_xs[x3]])
            ln_s1(L, i, xs[x3], t_xs[x3])

        def T2(i):
            ln_s2(L, i)

        def T3(i):
            ln_s3(L, i, xs[i % 3], t_xs[i % 3], lnin[:, 0:8], lnin[:, 8:16], i, False)
        pipeline(NT, [T0, T1, T2, T3], reverse=True)
        C.barrier()
        Sc.close()

    def load_w_cols(dst, tok, l, col0, ncols):
        src = w_in_d[l].rearrange("(c p) e -> p c e", p=128)[:, :, col0:col0 + ncols]
        C.dma(pool, dst[:], src, writes=[tok])

    def load_hbf(Sc):
        hb = Sc.sb([128, 8, S], BF16, "hbres")
        toks = [Tok() for _ in range(NT)]
        for i in range(NT):
            C.dma(sp, hb[:, :, i * TT:(i + 1) * TT], hbf_d[i], writes=[toks[i]])
        return hb, toks

    def phaseA(l, hb, hb_t):
        Sc = Scope(C)
        wq = [Sc.sb([128, 8, 384], BF16, "wqkv") for _ in range(2)]
        t_wq = [Tok(), Tok()]
        qT2 = [Sc.sb([128, S], BF16, "qT") for _ in range(2)]
        kT2 = [Sc.sb([128, S], BF16, "kT") for _ in range(2)]
        vv2 = [Sc.sb([128, 32, 128], BF16, "vv") for _ in range(2)]
        t_q2 = [[Tok() for _ in range(NT)] for _ in range(2)]
        t_k2 = [[Tok() for _ in range(NT)] for _ in range(2)]
        t_v2 = [[Tok() for _ in range(NT)] for _ in range(2)]
        W2 = 2 * TT
        NU, NSP, NE, NW = 2, 5, 3, 4
        u_sb = [Sc.sb([128, W2], F32, "u") for _ in range(NU)]
        sp_sb = [Sc.sb([128, W2], BF16, "spl") for _ in range(NSP)]
        e_sb = [Sc.sb([128, W2], F32, "e") for _ in range(NE)]
        w_sb = [Sc.sb([128, W2], BF16, "w") for _ in range(NW)]
        carry = [Sc.sb([128, W2], F32, "carry") for _ in range(2)]
        yo = [Sc.sb([128, TT], BF16, "yo") for _ in range(2)]
        t_u = [Tok() for _ in range(NU)]
        t_sp = [Tok() for _ in range(NSP)]
        t_e = [Tok() for _ in range(NE)]
        t_w = [Tok() for _ in range(NW)]
        t_carry = [Tok(), Tok()]
        t_yo = [Tok(), Tok()]
        _z = Sc.ps([128, W2], name="z")
        z_ps = [_z, _z]
        a_ps = Sc.ps([128, W2], name="a")
        c_ps = Sc.ps([128, W2], name="c")
        pv_ps = Sc.ps(name="pv")
        pj_ps = Sc.ps(name="pj")
        t_pj = Tok()
        _tz = Tok()
        t_z = [_tz, _tz]
        t_a = Tok()
        t_c = Tok()
        t_pv = Tok()

        def load_wq(p):
            for j in range(3):
                src = w_in_d[l].rearrange("(c p) e -> p c e", p=128)[:, :, j * 512 + p * 128: j * 512 + (p + 1) * 128]
                C.dma(pool, wq[p % 2][:, :, j * 128:(j + 1) * 128], src, writes=[t_wq[p % 2]])

        def proj_units(p):
            wb = p % 2
            qd, kd, vd = qT2[wb], kT2[wb], vv2[wb]
            units = []
            for i in range(NT):
                tsl = slice(i * TT, (i + 1) * TT)

                def uq(i=i, tsl=tsl):
                    C.mm(pj_ps[:], [(wq[wb][:, c, 0:128], hb[:, c, tsl]) for c in range(8)],
                         reads=[t_wq[wb], hb_t[i]], writes=[t_pj])
                    C.op(dve, lambda: nc.vector.tensor_scalar(out=qd[:, tsl], in0=pj_ps[:], scalar1=0.125, scalar2=None, op0=ALU.mult),
                         reads=[t_pj], writes=[t_q2[wb][i]])

                def uk(i=i, tsl=tsl):
                    C.mm(pj_ps[:], [(wq[wb][:, c, 128:256], hb[:, c, tsl]) for c in range(8)],
                         reads=[t_wq[wb], hb_t[i]], writes=[t_pj])
                    C.op(dve, lambda: nc.vector.tensor_copy(out=kd[:, tsl], in_=pj_ps[:]), reads=[t_pj], writes=[t_k2[wb][i]])

                def uv(i=i):
                    for tb in range(4):
                        blk = i * 4 + tb
                        C.mm(pj_ps[:, tb * 128:(tb + 1) * 128],
                             [(hb[:, c, blk * 128:(blk + 1) * 128], wq[wb][:, c, 256:384]) for c in range(8)],
                             reads=[t_wq[wb], hb_t[i]], writes=[t_pj])
                    C.op(dve, lambda: nc.vector.tensor_copy(out=vd[:, i * 4:(i + 1) * 4, :],
                                                            in_=pj_ps[:].rearrange("p (a b) -> p a b", b=128)),
                         reads=[t_pj], writes=[t_v2[wb][i]])
                units += [uq, uk, uv]
            return units

        load_wq(0)
        for u in proj_units(0):
            u()
        for p in range(4):
            wb = p % 2
            qT, kT, vv = qT2[wb], kT2[wb], vv2[wb]
            t_q, t_k, t_v = t_q2[wb], t_k2[wb], t_v2[wb]
            nxt = []
            if p + 1 < 4:
                load_wq(p + 1)
                nxt = proj_units(p + 1)
            steps = []
            sweep = 0
            for i in range(NT):
                nk = 4 * (i + 1)
                for m in range(nk):
                    kb = nk - 1 - m
                    steps.append(dict(i=i, kb=kb, first=(m == 0), last=(m == nk - 1), dj=(kb - 4 * i if kb >= 4 * i else None), sw=sweep))
                sweep += 1
            NST = len(steps)
            h0, h1 = slice(0, 64), slice(64, 128)

            def S1(n):
                T = steps[n]
                tsl = slice(T["i"] * TT, (T["i"] + 1) * TT)
                ks = slice(T["kb"] * 128, (T["kb"] + 1) * 128)
                ru, r = n % NU, n % NSP

                def fz():
                    nc.tensor.matmul(z_ps[0][:, 0:TT], kT[h0, ks], qT[h0, tsl], start=True, stop=True)
                    return nc.tensor.matmul(z_ps[0][:, TT:W2], kT[h1, ks], qT[h1, tsl], start=True, stop=True)
                C.op(pe, fz, reads=[t_k[T["kb"] // 4], t_q[T["i"]]], writes=[t_z[0]])
                C.op(act, lambda: nc.scalar.activation(out=u_sb[ru][:], in_=z_ps[0][:], func=AF.Exp), reads=[t_z[0]], writes=[t_u[ru]])
                C.op(act, lambda: nc.scalar.activation(out=sp_sb[r][:], in_=u_sb[ru][:], func=AF.Ln, bias=1.0),
                     reads=[t_u[ru]], writes=[t_sp[r]])
                if T["dj"] is not None:
                    m = masks[:, T["dj"] * W2:(T["dj"] + 1) * W2]
                    C.op(dve, lambda: nc.vector.tensor_tensor(out=sp_sb[r][:], in0=sp_sb[r][:], in1=m, op=ALU.mult),
                         reads=[t_sp[r]], writes=[t_sp[r]])

            def S2(n):
                T = steps[n]
                tsl = slice(T["i"] * TT, (T["i"] + 1) * TT)
                ks = slice(T["kb"] * 128, (T["kb"] + 1) * 128)
                r, re, rw = n % NSP, n % NE, n % NW
                cy = carry[T["sw"] % 2]
                t_cy = t_carry[T["sw"] % 2]
                sp1 = sp_sb[r][:, 0:TT]
                sp2 = sp_sb[r][:, TT:W2]

                def fa():
                    nc.tensor.matmul(a_ps[:, 0:TT], kT[h0, ks], qT[h0, tsl], start=True, stop=False)
                    nc.tensor.matmul(a_ps[:, TT:W2], kT[h1, ks], qT[h1, tsl], start=True, stop=False)
                    nc.tensor.matmul(a_ps[:, 0:TT], negtri[:], sp1, start=False, stop=True)
                    return nc.tensor.matmul(a_ps[:, TT:W2], negtri[:], sp2, start=False, stop=True)
                C.op(pe, fa, reads=[t_sp[r], t_k[T["kb"] // 4], t_q[T["i"]]], writes=[t_a])
                if not T["last"]:
                    def fc():
                        nc.tensor.matmul(c_ps[:, 0:TT], ones_bf[:], sp1, start=True, stop=True)
                        return nc.tensor.matmul(c_ps[:, TT:W2], ones_bf[:], sp2, start=True, stop=True)
                    C.op(pe, fc, reads=[t_sp[r]], writes=[t_c])
                if T["first"]:
                    C.op(dve, lambda: nc.vector.tensor_copy(out=e_sb[re][:], in_=a_ps[:]), reads=[t_a], writes=[t_e[re]])
                    if not T["last"]:
                        C.op(dve, lambda: nc.vector.tensor_scalar(out=cy[:], in0=c_ps[:], scalar1=-1.0, scalar2=None, op0=ALU.mult),
                             reads=[t_c], writes=[t_cy])
                else:
                    C.op(dve, lambda: nc.vector.tensor_tensor(out=e_sb[re][:], in0=a_ps[:], in1=cy[:], op=ALU.add),
                         reads=[t_a, t_cy], writes=[t_e[re]])
                    if not T["last"]:
                        C.op(dve, lambda: nc.vector.tensor_tensor(out=cy[:], in0=cy[:], in1=c_ps[:], op=ALU.subtract),
                             reads=[t_c, t_cy], writes=[t_cy])
                C.op(act, lambda: nc.scalar.activation(out=w_sb[rw][:], in_=e_sb[re][:], func=AF.Exp), reads=[t_e[re]], writes=[t_w[rw]])
                if T["dj"] is not None:
                    m = masks[:, T["dj"] * W2:(T["dj"] + 1) * W2]
                    C.op(pool, lambda: nc.gpsimd.tensor_tensor(out=w_sb[rw][:], in0=w_sb[rw][:], in1=m, op=ALU.mult),
                         reads=[t_w[rw]], writes=[t_w[rw]])

            def S3(n):
                T = steps[n]
                rw = n % NW
                pb = T["sw"] % 2

                def fp():
                    nc.tensor.matmul(pv_ps[0:64, :], vv[:, T["kb"], 0:64], w_sb[rw][:, 0:TT], start=T["first"], stop=T["last"])
                    return nc.tensor.matmul(pv_ps[64:128, :], vv[:, T["kb"], 64:128], w_sb[rw][:, TT:W2], start=T["first"], stop=T["last"])
                C.op(pe, fp, reads=[t_w[rw], t_v[T["kb"] // 4], t_pv], writes=[t_pv])
                if T["last"]:
                    C.op(dve, lambda: nc.vector.tensor_copy(out=yo[pb][:], in_=pv_ps[:]), reads=[t_pv], writes=[t_yo[pb]])
                    C.dma(sp, ysb_d[p * 128:(p + 1) * 128, T["i"] * TT:(T["i"] + 1) * TT], yo[pb][:], reads=[t_yo[pb]])

            for n in range(NST + 5):
                if n < NST:
                    S1(n)
                if 0 <= n - 3 < NST:
                    S2(n - 3)
                if 0 <= n - 5 < NST:
                    S3(n - 5)
                if nxt and n % 5 == 4:
                    nxt.pop(0)()
            while nxt:
                nxt.pop(0)()
        C.barrier()
        Sc.close()

    def phaseB(l, hb, hb_t):
        Sc = Scope(C)
        wl = [Sc.sb([128, 8, 256], BF16, "wl") for _ in range(2)]
        t_wl = [Tok(), Tok()]
        wab = [Sc.sb([128, 256], BF16, "wab") for _ in range(2)]
        t_wab = [Tok(), Tok()]
        lin = Sc.sb([128, 3 + S], F32, "lin")
        ub = Sc.sb([128, S], F32, "ub")
        rb_ = Sc.sb([128, S], F32, "rb")
        ib = Sc.sb([128, S], F32, "ib")
        ubf = Sc.sb([128, S], BF16, "ubf")
        yb = Sc.sb([128, S], BF16, "yb")
        NH = 2
        HS = S // NH
        t_lin = Tok()
        t_ub = [Tok() for _ in range(NH)]
        t_rb = [Tok() for _ in range(NH)]
        t_ib = [Tok() for _ in range(NH)]
        t_ubf = [Tok() for _ in range(NH)]
        t_yb = [Tok() for _ in range(NH)]
        ps = [Sc.ps(name="bps") for _ in range(4)]
        t_ps = [Tok() for _ in range(4)]
        C.op(dve, lambda: nc.vector.memset(lin[:, 0:3], 0.0), writes=[t_lin])
        for c in range(4):
            wbuf = c % 2
            src = w_in_d[l].rearrange("(c p) e -> p c e", p=128)
            C.dma(pool, wl[wbuf][:, :, 0:128], src[:, :, 1536 + c * 128:1536 + (c + 1) * 128], writes=[t_wl[wbuf]])
            C.dma(pool, wl[wbuf][:, :, 128:256], src[:, :, 2048 + c * 128:2048 + (c + 1) * 128], writes=[t_wl[wbuf]])
            C.dma(pool, wab[wbuf][:, 0:128], wa_d[l, c], writes=[t_wab[wbuf]])
            C.dma(pool, wab[wbuf][:, 128:256], wx_d[l, c], writes=[t_wab[wbuf]])
            for i in range(NT):
                tsl = slice(i * TT, (i + 1) * TT)
                k = i % 4
                C.mm(ps[k][:], [(wl[wbuf][:, cc, 0:128], hb[:, cc, tsl]) for cc in range(8)],
                     reads=[t_wl[wbuf], hb_t[i]], writes=[t_ps[k]])
                C.op(act, lambda: nc.scalar.activation(out=lin[:, 3 + i * TT:3 + (i + 1) * TT], in_=ps[k][:], func=AF.Copy),
                     reads=[t_ps[k]], writes=[t_lin])
            cw = lambda k: vcol(l, 56 + c * 4 + k)
            cc_ap = cchan[:, l * 4 + c:l * 4 + c + 1]
            for h in range(NH):
                hs = slice(h * HS, (h + 1) * HS)
                C.op(dve, lambda: nc.vector.tensor_scalar(out=ub[:, hs], in0=lin[:, h * HS:h * HS + HS], scalar1=cw(0), scalar2=vcol(l, 72 + c),
                                                          op0=ALU.mult, op1=ALU.add), reads=[t_lin], writes=[t_ub[h]])
                for k in range(1, 4):
                    C.op(dve, lambda: nc.vector.scalar_tensor_tensor(out=ub[:, hs], in0=lin[:, h * HS + k:h * HS + k + HS], scalar=cw(k),
                                                                     in1=ub[:, hs], op0=ALU.mult, op1=ALU.add),
                         reads=[t_lin, t_ub[h]], writes=[t_ub[h]])
                C.op(act, lambda: nc.scalar.activation(out=ubf[:, hs], in_=ub[:, hs], func=AF.Copy), reads=[t_ub[h]], writes=[t_ubf[h]])
            for h in range(NH):
                for i in range(h * (NT // NH), (h + 1) * (NT // NH)):
                    tsl = slice(i * TT, (i + 1) * TT)
                    k = i % 2
                    C.mm(ps[k][:], [(wab[wbuf][:, 0:128], ubf[:, tsl])], reads=[t_wab[wbuf], t_ubf[h]], writes=[t_ps[k]])
                    C.op(act, lambda: nc.scalar.activation(out=rb_[:, tsl], in_=ps[k][:], func=AF.Sigmoid, bias=vcol(l, 76 + c)),
                         reads=[t_ps[k]], writes=[t_rb[h]])
                    C.mm(ps[2 + k][:], [(wab[wbuf][:, 128:256], ubf[:, tsl])], reads=[t_wab[wbuf], t_ubf[h]], writes=[t_ps[2 + k]])
                    C.op(act, lambda: nc.scalar.activation(out=ib[:, tsl], in_=ps[2 + k][:], func=AF.Sigmoid, bias=vcol(l, 80 + c)),
                         reads=[t_ps[2 + k]], writes=[t_ib[h]])
            for h in range(NH):
                hs = slice(h * HS, (h + 1) * HS)
                C.op(act, lambda: nc.scalar.activation(out=rb_[:, hs], in_=rb_[:, hs], func=AF.Exp, scale=cc_ap),
                     reads=[t_rb[h]], writes=[t_rb[h]])
            for h in range(NH):
                hs = slice(h * HS, (h + 1) * HS)
                C.op(dve, lambda: nc.vector.tensor_tensor(out=ib[:, hs], in0=ib[:, hs], in1=ub[:, hs], op=ALU.mult),
                     reads=[t_ib[h], t_ub[h]], writes=[t_ib[h]])
                C.op(dve, lambda: nc.vector.tensor_tensor(out=ub[:, hs], in0=rb_[:, hs], in1=rb_[:, hs], op=ALU.mult),
                     reads=[t_rb[h], t_ib[h], t_ubf[h]], writes=[t_ub[h]])
                C.op(dve, lambda: nc.vector.tensor_scalar(out=ub[:, hs], in0=ub[:, hs], scalar1=-1.0, scalar2=1.0, op0=ALU.mult, op1=ALU.add),
                     reads=[t_ub[h]], writes=[t_ub[h]])
            for h in range(NH):
                hs = slice(h * HS, (h + 1) * HS)
                C.op(act, lambda: nc.scalar.activation(out=ub[:, hs], in_=ub[:, hs], func=AF.Sqrt), reads=[t_ub[h]], writes=[t_ub[h]])
            for h in range(NH):
                hs = slice(h * HS, (h + 1) * HS)
                C.op(dve, lambda: nc.vector.tensor_tensor(out=ib[:, hs], in0=ib[:, hs], in1=ub[:, hs], op=ALU.mult),
                     reads=[t_ib[h], t_ub[h]], writes=[t_ib[h]])
                init = 0.0 if h == 0 else ub[:, h * HS - 1:h * HS]
                rd = [t_rb[h], t_ib[h], t_ub[h]] + ([t_ub[h - 1]] if h > 0 else [])
                C.op(dve, lambda: nc.vector.tensor_tensor_scan(out=ub[:, hs], data0=rb_[:, hs], data1=ib[:, hs], initial=init,
                                                               op0=ALU.mult, op1=ALU.add), reads=rd, writes=[t_ub[h]])
            for h in range(NH):
                hs = slice(h * HS, (h + 1) * HS)
                for i in range(h * (NT // NH), (h + 1) * (NT // NH)):
                    tsl = slice(i * TT, (i + 1) * TT)
                    k = i % 4
                    C.mm(ps[k][:], [(wl[wbuf][:, cc, 128:256], hb[:, cc, tsl]) for cc in range(8)],
                         reads=[t_wl[wbuf], hb_t[i]], writes=[t_ps[k]])
                    C.op(act, lambda: nc.scalar.activation(out=rb_[:, tsl], in_=ps[k][:], func=AF.Gelu_apprx_tanh),
                         reads=[t_ps[k], t_ub[h]], writes=[t_rb[h]])
                C.op(dve, lambda: nc.vector.tensor_tensor(out=yb[:, hs], in0=rb_[:, hs], in1=ub[:, hs], op=ALU.mult),
                     reads=[t_rb[h], t_ub[h]], writes=[t_yb[h]])
                C.dma(sp, ylru_d[c * 128:(c + 1) * 128, hs], yb[:, hs], reads=[t_yb[h]])
        C.barrier()
        Sc.close()

    def phaseC(l, hb, hb_t):
        Sc = Scope(C)
        wl = [Sc.sb([128, 8, 384], BF16, "wc") for _ in range(2)]
        t_wl = [Tok(), Tok()]
        cb = [Sc.sb([128, S], F32, "ccb") for _ in range(2)]
        ph = [Sc.sb([128, 2 + S], F32, "cph") for _ in range(2)]
        vb = Sc.sb([128, S], F32, "cvb")
        yb = Sc.sb([128, S], BF16, "cyb")
        t_cb, t_ph = [Tok(), Tok()], [Tok(), Tok()]
        t_vb, t_yb = Tok(), Tok()
        ps = [Sc.ps(name="cps") for _ in range(4)]
        t_ps = [Tok() for _ in range(4)]
        for b in range(2):
            C.op(dve, lambda: nc.vector.memset(ph[b][:, 0:2], 0.0), writes=[t_ph[b]])
        src = w_in_d[l].rearrange("(c p) e -> p c e", p=128)

        def front(c):
            wbuf = c % 2
            for j in range(3):
                C.dma(pool, wl[wbuf][:, :, j * 128:(j + 1) * 128], src[:, :, 2560 + j * 512 + c * 128:2560 + j * 512 + (c + 1) * 128],
                      writes=[t_wl[wbuf]])
            for i in range(NT):
                tsl = slice(i * TT, (i + 1) * TT)
                k = i % 2
                C.mm(ps[k][:], [(wl[wbuf][:, cc, 128:256], hb[:, cc, tsl]) for cc in range(8)],
                     reads=[t_wl[wbuf], hb_t[i]], writes=[t_ps[k]])
                C.op(act, lambda: nc.scalar.activation(out=cb[wbuf][:, tsl], in_=ps[k][:], func=AF.Copy), reads=[t_ps[k]], writes=[t_cb[wbuf]])
                C.mm(ps[2 + k][:], [(wl[wbuf][:, cc, 256:384], hb[:, cc, tsl]) for cc in range(8)],
                     reads=[t_wl[wbuf], hb_t[i]], writes=[t_ps[2 + k]])
                C.op(dve, lambda: nc.vector.tensor_tensor(out=ph[wbuf][:, 2 + i * TT:2 + (i + 1) * TT], in0=ps[2 + k][:], in1=cb[wbuf][:, tsl],
                                                          op=ALU.mult), reads=[t_ps[2 + k], t_cb[wbuf]], writes=[t_ph[wbuf]])

        def back(c):
            wbuf = c % 2
            cw = lambda k: vcol(l, 88 + c * 3 + k)
            C.op(dve, lambda: nc.vector.tensor_scalar(out=vb[:], in0=ph[wbuf][:, 0:S], scalar1=cw(0), scalar2=None, op0=ALU.mult),
                 reads=[t_ph[wbuf], t_yb], writes=[t_vb])
            for k in range(1, 3):
                C.op(dve, lambda: nc.vector.scalar_tensor_tensor(out=vb[:], in0=ph[wbuf][:, k:k + S], scalar=cw(k), in1=vb[:],
                                                                 op0=ALU.mult, op1=ALU.add), reads=[t_ph[wbuf], t_vb], writes=[t_vb])
            for i in range(NT):
                tsl = slice(i * TT, (i + 1) * TT)
                k = i % 2
                C.mm(ps[k][:], [(wl[wbuf][:, cc, 0:128], hb[:, cc, tsl]) for cc in range(8)],
                     reads=[t_wl[wbuf], hb_t[i]], writes=[t_ps[k]])
                C.op(dve, lambda: nc.vector.tensor_tensor(out=yb[:, tsl], in0=ps[k][:], in1=vb[:, tsl], op=ALU.mult),
                     reads=[t_ps[k], t_vb], writes=[t_yb])
            C.dma(sp, ysc_d[c * 128:(c + 1) * 128, :], yb[:], reads=[t_yb])

        front(0)
        for c in range(4):
            if c + 1 < 4:
                front(c + 1)
            back(c)
        C.barrier()
        Sc.close()

    def phaseD1(l, hb, hb_t):
        Sc = Scope(C)
        wg = Sc.sb([128, 8, 3072], BF16, "wgate")
        wbr = Sc.sb([128, 12, D], BF16, "wbr")
        t_wg, t_wbr = Tok(), Tok()
        src = w_in_d[l].rearrange("(c p) e -> p c e", p=128)
        for j in range(6):
            C.dma(pool, wg[:, :, j * 512:(j + 1) * 512], src[:, :, 4096 + j * 512:4096 + (j + 1) * 512], writes=[t_wg])
        for br in range(3):
            C.dma(pool, wbr[:, br * 4:(br + 1) * 4, :], wbr_d[l, br].rearrange("(c p) d -> p c d", p=128), writes=[t_wbr])
        yt = [Sc.sb([128, 12, TT], BF16, "yt") for _ in range(2)]
        t_yt = [Tok(), Tok()]
        mt = [Sc.sb([128, 8, TT], BF16, "mt")] * 2
        _tm = Tok()
        t_mt = [_tm, _tm]
        sg = [Sc.sb([128, TT], F32, "sg") for _ in range(3)]
        t_sg = [Tok() for _ in range(3)]
        acc = [Sc.sb([128, TT], F32, "macc") for _ in range(2)]
        t_acc = [Tok(), Tok()]
        tmp = [Sc.sb([128, TT], F32, "mtmp") for _ in range(2)]
        t_tmp = [Tok(), Tok()]
        gps = [Sc.ps(name="gps") for _ in range(3)]
        pps = [Sc.ps(name="pps") for _ in range(3)]
        t_g = [Tok() for _ in range(3)]
        t_p = [Tok() for _ in range(3)]
        ysrc = [ysb_d, ylru_d, ysc_d]
        def load_y(i):
            for br in range(3):
                C.dma(sp, yt[i % 2][:, br * 4:(br + 1) * 4, :],
                      ysrc[br].rearrange("(c p) t -> p c t", p=128)[:, :, i * TT:(i + 1) * TT], writes=[t_yt[i % 2]])
        load_y(0)
        for i in range(NT):
            tsl = slice(i * TT, (i + 1) * TT)
            b = i % 2
            if i + 1 < NT:
                load_y(i + 1)
            for dc in range(8):
                a = dc % 2
                for br in range(3):
                    col = br * 1024 + dc * 128
                    C.mm(gps[br][:], [(wg[:, cc, col:col + 128], hb[:, cc, tsl]) for cc in range(8)],
                         reads=[t_wg, hb_t[i]], writes=[t_g[br]])
                    C.op(act, lambda: nc.scalar.activation(out=sg[br][:], in_=gps[br][:], func=AF.Sigmoid, bias=vcol(l, 32 + br * 8 + dc)),
                         reads=[t_g[br]], writes=[t_sg[br]])
                    C.mm(pps[br][:], [(wbr[:, br * 4 + wc, dc * 128:(dc + 1) * 128], yt[b][:, br * 4 + wc, :]) for wc in range(4)],
                         reads=[t_wbr, t_yt[b]], writes=[t_p[br]])
                C.op(dve, lambda: nc.vector.tensor_tensor(out=acc[a][:], in0=pps[0][:], in1=sg[0][:], op=ALU.mult),
                     reads=[t_p[0], t_sg[0]], writes=[t_acc[a]])
                C.op(dve, lambda: nc.vector.tensor_tensor(out=tmp[0][:], in0=pps[1][:], in1=sg[1][:], op=ALU.mult),
                     reads=[t_p[1], t_sg[1]], writes=[t_tmp[0]])
                C.op(dve, lambda: nc.vector.tensor_tensor(out=tmp[1][:], in0=pps[2][:], in1=sg[2][:], op=ALU.mult),
                     reads=[t_p[2], t_sg[2]], writes=[t_tmp[1]])
                C.op(pool, lambda: nc.gpsimd.tensor_tensor(out=acc[a][:], in0=acc[a][:], in1=tmp[0][:], op=ALU.add),
                     reads=[t_tmp[0], t_acc[a]], writes=[t_acc[a]])
                C.op(pool, lambda: nc.gpsimd.tensor_tensor(out=mt[b][:, dc, :], in0=acc[a][:], in1=tmp[1][:], op=ALU.add),
                     reads=[t_tmp[1], t_acc[a]], writes=[t_mt[b]])
            C.dma(sp, mT_d[i], mt[b][:], reads=[t_mt[b]])
        C.barrier()
        Sc.close()

    def phaseD2(l):
        Sc = Scope(C)
        L = ln_alloc(Sc, 2)
        lps = [Sc.ps(name="lps") for _ in range(2)]
        t_lps = [Tok(), Tok()]

        def lg_copy(i):
            C.op(dve, lambda: nc.vector.tensor_copy(out=lg[:, i * 4:(i + 1) * 4, :],
                                                    in_=lps[i % 2][:, 0:128].rearrange("p (a b) -> p a b", b=32)[:, :, 0:20]),
                 reads=[t_lps[i % 2]], writes=[t_lg])
        C.dma(sp, wr_sb[:], wr_d[l].rearrange("(c p) e -> p c e", p=128), writes=[t_wr])
        wo = Sc.sb([128, 8, D], BF16, "wo")
        t_wo = Tok()
        C.dma(pool, wo[:], wout_d[l].rearrange("(c p) e -> p c e", p=128), writes=[t_wo])
        NX = 4
        mt = [Sc.sb([128, 8, TT], BF16, "mt2") for _ in range(3)]
        t_mt = [Tok() for _ in range(3)]
        xs = [Sc.sb([128, 8, TT], F32, "xs2") for _ in range(NX)]
        t_xs = [Tok() for _ in range(NX)]
        ops_ = [Sc.ps(name="ops") for _ in range(2)]
        t_o = [Tok(), Tok()]

        def T0(i):
            C.dma(sp, mt[i % 3][:], mT_d[i], writes=[t_mt[i % 3]])
            C.dma(sp, xs[i % NX][:], h32_d[i], writes=[t_xs[i % NX]])

        def T1(i):
            b = i % 3
            x3 = i % NX
            for ec in range(8):
                k = ec % 2
                C.mm(ops_[k][:], [(wo[:, dc, ec * 128:(ec + 1) * 128], mt[b][:, dc, :]) for dc in range(8)],
                     reads=[t_wo, t_mt[b]], writes=[t_o[k]])
                C.op(dve, lambda: nc.vector.scalar_tensor_tensor(out=xs[x3][:, ec, :], in0=xs[x3][:, ec, :], scalar=ALPHA, in1=ops_[k][:],
                                                                 op0=ALU.mult, op1=ALU.add), reads=[t_o[k], t_xs[x3]], writes=[t_xs[x3]])
            ln_s1(L, i, xs[x3], t_xs[x3])

        def T2(i):
            ln_s2(L, i)

        def T3(i):
            ln_s3(L, i, xs[i % NX], t_xs[i % NX], vcol(l, 0, 8), vcol(l, 8, 8), i, False,
                  router=(wr_sb, t_wr, lps, t_lps, lg, t_lg, lg_copy))
        pipeline(NT, [T0, T1, T2, T3], reverse=True)
        lg_copy(NT - 1)
        C.barrier()
        Sc.close()

    def phaseE(l, final):
        Sm = Scope(C)
        cT = Sm.sb([16, S], BF16, "cT")
        t_cT = Tok()
        ST = 2048
        oacc = Sm.sb([128, 8, ST], F32, "oacc")
        Sr = Scope(C)
        lps = [Sr.ps(name="lps") for _ in range(2)]
        t_lps = [Tok(), Tok()]
        NB = 32
        t_r = Tok()

        def rt(shape, name):
            return Sr.sb(shape, F32, name)
        glb = rt([128, NB, 4], "glb")
        gmx = rt([128, NB], "gmx")
        ohg = rt([128, NB, 4], "ohg")
        eg = rt([128, NB, 4], "eg")
        gs = rt([128, NB], "gs")
        psel = rt([128, NB], "psel")
        t44 = rt([128, NB, 4, 4], "t44")
        ein = rt([128, NB, 4], "ein")
        sc = rt([128, NB, 4], "sc")
        m1 = rt([128, NB], "m1")
        oh1 = rt([128, NB, 4], "oh1")
        oh2 = rt([128, NB, 4], "oh2")
        l1 = rt([128, NB], "l1")
        l2 = rt([128, NB], "l2")
        w1 = rt([128, NB], "w1")
        w2 = rt([128, NB], "w2")
        cw = rt([128, NB, 4], "cw")
        comb = rt([128, NB, 4, 4], "comb")
        gl = lg[:, :, 0:4]
        el4 = lg[:, :, 4:20].rearrange("p b (g e) -> p b g e", e=4)
        gbias = rbias[:, l * 20:l * 20 + 4]
        ebias = rbias[:, l * 20 + 4:l * 20 + 20].rearrange("p (g e) -> p g e", e=4)

        def bc3(ap2):
            return ap2.unsqueeze(2).to_broadcast([128, NB, 4])

        def V(fn, reads=(t_lg,)):
            C.op(dve, fn, reads=list(reads) + [t_r], writes=[t_r])

        V(lambda: nc.vector.tensor_tensor(out=glb[:], in0=gl, in1=gbias.unsqueeze(1).to_broadcast([128, NB, 4]), op=ALU.add))
        V(lambda: nc.vector.tensor_reduce(out=gmx[:], in_=glb[:], axis=AX.X, op=ALU.max))
        V(lambda: nc.vector.tensor_tensor(out=ohg[:], in0=glb[:], in1=bc3(gmx[:]), op=ALU.is_equal))
        V(lambda: nc.vector.tensor_reduce(out=gmx[:], in_=gl, axis=AX.X, op=ALU.max))
        V(lambda: nc.vector.tensor_tensor(out=eg[:], in0=gl, in1=bc3(gmx[:]), op=ALU.subtract))
        C.op(act, lambda: nc.scalar.activation(out=eg[:], in_=eg[:], func=AF.Exp), reads=[t_r], writes=[t_r])
        V(lambda: nc.vector.tensor_reduce(out=gs[:], in_=eg[:], axis=AX.X, op=ALU.add))
        V(lambda: nc.vector.tensor_tensor(out=eg[:], in0=eg[:], in1=ohg[:], op=ALU.mult))
        V(lambda: nc.vector.tensor_reduce(out=psel[:], in_=eg[:], axis=AX.X, op=ALU.add))
        V(lambda: nc.vector.reciprocal(out=gs[:], in_=gs[:]))
        V(lambda: nc.vector.tensor_tensor(out=psel[:], in0=psel[:], in1=gs[:], op=ALU.mult))
        ohg4 = ohg[:].unsqueeze(3).to_broadcast([128, NB, 4, 4])
        V(lambda: nc.vector.tensor_tensor(out=t44[:], in0=el4, in1=ohg4, op=ALU.mult))
        V(lambda: nc.vector.tensor_reduce(out=ein[:], in_=t44[:].rearrange("p b g e -> p b e g"), axis=AX.X, op=ALU.add))
        V(lambda: nc.vector.tensor_tensor(out=t44[:], in0=ohg4, in1=ebias.unsqueeze(1).to_broadcast([128, NB, 4, 4]), op=ALU.mult))
        V(lambda: nc.vector.tensor_reduce(out=sc[:], in_=t44[:].rearrange("p b g e -> p b e g"), axis=AX.X, op=ALU.add))
        V(lambda: nc.vector.tensor_tensor(out=sc[:], in0=sc[:], in1=ein[:], op=ALU.add))
        V(lambda: nc.vector.tensor_reduce(out=m1[:], in_=sc[:], axis=AX.X, op=ALU.max))
        V(lambda: nc.vector.tensor_tensor(out=oh1[:], in0=sc[:], in1=bc3(m1[:]), op=ALU.is_equal))
        V(lambda: nc.vector.scalar_tensor_tensor(out=sc[:], in0=oh1[:], scalar=-1e30, in1=sc[:], op0=ALU.mult, op1=ALU.add))
        V(lambda: nc.vector.tensor_reduce(out=m1[:], in_=sc[:], axis=AX.X, op=ALU.max))
        V(lambda: nc.vector.tensor_tensor(out=oh2[:], in0=sc[:], in1=bc3(m1[:]), op=ALU.is_equal))
        V(lambda: nc.vector.tensor_tensor(out=sc[:], in0=oh1[:], in1=ein[:], op=ALU.mult))
        V(lambda: nc.vector.tensor_reduce(out=l1[:], in_=sc[:], axis=AX.X, op=ALU.add))
        V(lambda: nc.vector.tensor_tensor(out=sc[:], in0=oh2[:], in1=ein[:], op=ALU.mult))
        V(lambda: nc.vector.tensor_reduce(out=l2[:], in_=sc[:], axis=AX.X, op=ALU.add))
        V(lambda: nc.vector.tensor_tensor(out=l1[:], in0=l1[:], in1=l2[:], op=ALU.subtract))
        C.op(act, lambda: nc.scalar.activation(out=w1[:], in_=l1[:], func=AF.Sigmoid), reads=[t_r], writes=[t_r])
        V(lambda: nc.vector.tensor_tensor(out=w1[:], in0=w1[:], in1=psel[:], op=ALU.mult))
        V(lambda: nc.vector.tensor_tensor(out=w2[:], in0=psel[:], in1=w1[:], op=ALU.subtract))
        V(lambda: nc.vector.tensor_tensor(out=cw[:], in0=oh1[:], in1=bc3(w1[:]), op=ALU.mult))
        V(lambda: nc.vector.tensor_tensor(out=oh2[:], in0=oh2[:], in1=bc3(w2[:]), op=ALU.mult))
        V(lambda: nc.vector.tensor_tensor(out=cw[:], in0=cw[:], in1=oh2[:], op=ALU.add))
        V(lambda: nc.vector.tensor_tensor(out=comb[:], in0=ohg4, in1=cw[:].unsqueeze(2).to_broadcast([128, NB, 4, 4]), op=ALU.mult))
        for i in range(NT):
            b = i % 2
            for tb in range(4):
                blk = i * 4 + tb
                C.mm(lps[b][0:16, tb * 128:(tb + 1) * 128], [(comb[:, blk].rearrange("p g e -> p (g e)"), ident[:])],
                     reads=[t_r], writes=[t_lps[b]])
            C.op(act, lambda: nc.scalar.activation(out=cT[:, i * TT:(i + 1) * TT], in_=lps[b][0:16, :], func=AF.Copy),
                 reads=[t_lps[b]], writes=[t_cT])
        if debug:
            C.dma(sp, dbg_comb[:, :, :], comb[:].rearrange("p b g e -> p b (g e)"), reads=[t_r])
        C.barrier()
        Sr.close()

        t_hbs = [Tok() for _ in range(4)]
        t_oacc = [[Tok() for _ in range(8)] for _ in range(4)]
        for st in range(2):
            Se = Scope(C)
            hbs = Se.sb([128, 8, ST], BF16, "hbs")
            for t in range(4):
                g0 = st * ST + t * TT
                C.dma(sp, hbs[:, :, t * TT:(t + 1) * TT], hbf_d[st * 4 + t], writes=[t_hbs[t]])
            wgu = [Se.sb([128, 16, 512], BF16, "wgu") for _ in range(2)]
            wdn = [Se.sb([128, 4, D], BF16, "wdn") for _ in range(2)]
            t_wgu = [Tok(), Tok()]
            t_wdn = [Tok(), Tok()]
            cbs = [Se.sb([128, TT], F32, "cbs") for _ in range(2)]
            t_cbs = [Tok(), Tok()]
            sgb = [Se.sb([128, TT], F32, "sgb") for _ in range(2)]
            t_sgb = [Tok(), Tok()]
            t1b = [Se.sb([128, TT], F32, "t1b") for _ in range(2)]
            t_t1b = [Tok(), Tok()]
            hc = [Se.sb([128, 4, TT], BF16, "hc") for _ in range(2)]
            t_hc = [Tok(), Tok()]
            gp = [Se.ps(name="gp") for _ in range(2)]
            up = [Se.ps(name="up") for _ in range(2)]
            dp = [Se.ps(name="dp") for _ in range(2)]
            cbp = Se.ps(name="cbp")
            t_gp = [Tok(), Tok()]
            t_up = [Tok(), Tok()]
            t_dp = [Tok(), Tok()]
            t_cbp = Tok()

            def load_expert(e):
                wbuf = e % 2
                C.dma(pool, wgu[wbuf][:, 0:8, :], wg_d[l, e].rearrange("(c p) f -> p c f", p=128), writes=[t_wgu[wbuf]])
                C.dma(pool, wgu[wbuf][:, 8:16, :], wu_d[l, e].rearrange("(c p) f -> p c f", p=128), writes=[t_wgu[wbuf]])
                C.dma(pool, wdn[wbuf][:], wd_d[l, e].rearrange("(c p) d -> p c d", p=128), writes=[t_wdn[wbuf]])

            load_expert(0)

            def GU(n):
                e, t = n // 4, n % 4
                wbuf = e % 2
                if t == 1 and e + 1 < 16:
                    load_expert(e + 1)
                g0 = st * ST + t * TT
                tl = slice(t * TT, (t + 1) * TT)
                hb_i = n % 2
                C.mm(cbp[:], [(sel[:, e * 128:(e + 1) * 128], cT[:, g0:g0 + TT])], reads=[t_cT], writes=[t_cbp])
                C.op(act, lambda: nc.scalar.activation(out=cbs[hb_i][:], in_=cbp[:], func=AF.Copy), reads=[t_cbp], writes=[t_cbs[hb_i]])
                for fc in range(4):
                    k = fc % 2
                    C.mm(gp[k][:], [(wgu[wbuf][:, c, fc * 128:(fc + 1) * 128], hbs[:, c, tl]) for c in range(8)],
                         reads=[t_wgu[wbuf], t_hbs[t]], writes=[t_gp[k]])
                    C.mm(up[k][:], [(wgu[wbuf][:, 8 + c, fc * 128:(fc + 1) * 128], hbs[:, c, tl]) for c in range(8)],
                         reads=[t_wgu[wbuf], t_hbs[t]], writes=[t_up[k]])
                    C.op(act, lambda: nc.scalar.activation(out=sgb[k][:], in_=gp[k][:], func=AF.Silu), reads=[t_gp[k]], writes=[t_sgb[k]])
                    C.op(dve, lambda: nc.vector.tensor_tensor(out=t1b[k][:], in0=up[k][:], in1=sgb[k][:], op=ALU.mult),
                         reads=[t_up[k], t_sgb[k]], writes=[t_t1b[k]])
                    C.op(pool, lambda: nc.gpsimd.tensor_tensor(out=hc[hb_i][:, fc, :], in0=t1b[k][:], in1=cbs[hb_i][:], op=ALU.mult),
                         reads=[t_t1b[k], t_cbs[hb_i]], writes=[t_hc[hb_i]])

            def DN(n):
                e, t = n // 4, n % 4
                wbuf = e % 2
                tl = slice(t * TT, (t + 1) * TT)
                hb_i = n % 2
                for dc in range(8):
                    k = dc % 2
                    C.mm(dp[k][:], [(wdn[wbuf][:, fc, dc * 128:(dc + 1) * 128], hc[hb_i][:, fc, :]) for fc in range(4)],
                         reads=[t_wdn[wbuf], t_hc[hb_i]], writes=[t_dp[k]])
                    if e == 0:
                        C.op(act, lambda: nc.scalar.activation(out=oacc[:, dc, tl], in_=dp[k][:], func=AF.Copy),
                             reads=[t_dp[k]], writes=[t_oacc[t][dc]])
                    else:
                        C.op(dve, lambda: nc.vector.tensor_tensor(out=oacc[:, dc, tl], in0=oacc[:, dc, tl], in1=dp[k][:], op=ALU.add),
                             reads=[t_dp[k], t_oacc[t][dc]], writes=[t_oacc[t][dc]])
            pipeline(64, [GU, DN])
            C.barrier()
            Se.close()
            Sl = Scope(C)
            L = ln_alloc(Sl, 2, final)
            xs = [Sl.sb([128, 8, TT], F32, "xs3") for _ in range(3)]
            t_xs = [Tok() for _ in range(3)]

            def T0(t):
                C.dma(sp, xs[t % 3][:], h32_d[st * 4 + t], writes=[t_xs[t % 3]])

            def T1(t):
                x3 = t % 3
                tl = slice(t * TT, (t + 1) * TT)
                C.op(dve, lambda: nc.vector.scalar_tensor_tensor(out=xs[x3][:], in0=xs[x3][:], scalar=ALPHA, in1=oacc[:, :, tl],
                                                                 op0=ALU.mult, op1=ALU.add), reads=[t_xs[x3]] + t_oacc[t], writes=[t_xs[x3]])
                ln_s1(L, t, xs[x3], t_xs[x3])

            def T2(t):
                ln_s2(L, t)

            def T3(t):
                ln_s3(L, t, xs[t % 3], t_xs[t % 3], vcol(l, 16, 8), vcol(l, 24, 8), st * 4 + t, final)
            pipeline(4, [T0, T1, T2, T3], reverse=True)
            C.barrier()
            Sl.close()
        Sm.close()

    dbg_comb = None
    if debug:
        dbg_comb = nc.dram_tensor("dbg_comb", [128, 32, 16], F32, kind="ExternalOutput").ap()

    def run():
        phase0()
        if stop == "p0":
            return
        for l in range(nlayers):
            Sh = Scope(C)
            hb, hb_t = load_hbf(Sh)
            phaseA(l, hb, hb_t)
            if stop == "A":
                Sh.close()
                return
            phaseB(l, hb, hb_t)
            if stop == "B":
                Sh.close()
                return
            phaseC(l, hb, hb_t)
            if stop == "C":
                Sh.close()
                return
            phaseD1(l, hb, hb_t)
            Sh.close()
            if stop == "D1":
                return
            phaseD2(l)
            if stop == "D2":
                return
            phaseE(l, final=(l == nlayers - 1 and stop is None))
            if stop == "E":
                return

    run()
    C.barrier()
    G.close()
    C.es.close()
    return nc


def _fmaj(v, n):
    return np.ascontiguousarray(np.asarray(v, np.float32).reshape(n, 128).T)


def prepare_inputs(inputs):
    f32 = np.float32
    g = {k: np.asarray(v) for k, v in inputs.items()}
    vecs = np.zeros((DEPTH, 128, NV), f32)
    rb = np.zeros((DEPTH, 128, 20), f32)
    wa_bd = np.zeros((DEPTH, 4, 128, 128), f32)
    wx_bd = np.zeros((DEPTH, 4, 128, 128), f32)
    for l in range(DEPTH):
        vecs[l, :, 0:8] = _fmaj(g["ln1_g"][l], 8)
        vecs[l, :, 8:16] = _fmaj(g["ln1_b"][l], 8)
        vecs[l, :, 16:24] = _fmaj(g["ln2_g"][l], 8)
        vecs[l, :, 24:32] = _fmaj(g["ln2_b"][l], 8)
        for br in range(3):
            vecs[l, :, 32 + br * 8:40 + br * 8] = _fmaj(g["gate_b"][l, br], 8)
        for k in range(4):
            vecs[l, :, 56 + k:72:4] = _fmaj(g["lru_conv_w"][l, k], 4)
        vecs[l, :, 72:76] = _fmaj(g["lru_conv_b"][l], 4)
        vecs[l, :, 76:80] = _fmaj(g["lru_ba"][l], 4)
        vecs[l, :, 80:84] = _fmaj(g["lru_bx"][l], 4)
        vecs[l, :, 84:88] = _fmaj(g["lru_lambda"][l], 4)
        for k in range(3):
            vecs[l, :, 88 + k:100:3] = _fmaj(g["sc_conv_w"][l, k], 4)
        rb[l, :, 0:4] = g["group_bias"][l][None, :]
        rb[l, :, 4:20] = g["expert_bias"][l][None, :]
        for c in range(4):
            for h in range(2):
                wa_bd[l, c, h * 64:(h + 1) * 64, h * 64:(h + 1) * 64] = g["lru_wa"][l, 2 * c + h]
                wx_bd[l, c, h * 64:(h + 1) * 64, h * 64:(h + 1) * 64] = g["lru_wx"][l, 2 * c + h]
    lnin = np.concatenate([_fmaj(g["ln_in_g"], 8), _fmaj(g["ln_in_b"], 8)], axis=1)
    w_branch = np.ascontiguousarray(np.stack([g["w_branch_sb"], g["w_branch_lru"], g["w_branch_sc"]], axis=1).astype(f32))
    w_router = np.ascontiguousarray(np.concatenate([g["w_group"], g["w_expert_router"]], axis=2).astype(f32))
    ident = np.eye(128, dtype=f32)
    jj, ss = np.meshgrid(np.arange(128), np.arange(128), indexing="ij")
    negtri = np.where(jj >= ss, -1.0, 0.0).astype(f32)
    sidx = np.arange(128)[:, None]
    tidx = np.arange(512)[None, :]
    masks = np.concatenate([(tidx > sidx + 128 * j).astype(f32) for j in (0, 0, 1, 1, 2, 2, 3, 3)], axis=1)
    sel = np.zeros((16, 16, 128), f32)
    for e in range(16):
        sel[e, e, :] = 1.0
    sel = sel.reshape(16, 16 * 128)
    shared = dict(w_in=np.ascontiguousarray(g["w_in"].astype(f32)), wa_bd=wa_bd, wx_bd=wx_bd, w_branch=w_branch,
                  w_out=np.ascontiguousarray(g["w_out"].astype(f32)), w_router=w_router,
                  w_gate=np.ascontiguousarray(g["w_gate"].astype(f32)), w_up=np.ascontiguousarray(g["w_up"].astype(f32)),
                  w_down=np.ascontiguousarray(g["w_down"].astype(f32)), vecs=vecs, lnin=np.ascontiguousarray(lnin),
                  rbias=rb, ident=ident, negtri=negtri, masks=np.ascontiguousarray(masks), sel=sel)
    x = np.asarray(g["x"], f32)
    in_maps = []
    for c in range(NCORES):
        m = dict(shared)
        m["x"] = np.ascontiguousarray(x[c])
        in_maps.append(m)
    return in_maps


def kernel(**inputs):
    in_maps = prepare_inputs(inputs)
    nc = build()
    res = run_bass_kernel_spmd(nc, in_maps, core_ids=list(range(NCORES)))
    return np.stack([np.asarray(res.results[c]["out"], np.float32) for c in range(NCORES)], axis=0)
```

```python
import numpy as np
from contextlib import ExitStack
import concourse.bass as bass
import concourse.mybir as mybir
from concourse.bass_utils import run_bass_kernel_spmd

F32 = mybir.dt.float32
BF16 = mybir.dt.bfloat16
AF = mybir.ActivationFunctionType
ALU = mybir.AluOpType
AX = mybir.AxisListType

S = 4096
D = 1024
PC = 7168
NT = 8
TT = 512
DEPTH = 2
ALPHA = float((2 * DEPTH) ** 0.25)
LN_EPS = 1e-5
NV = 100
NCORES = 8


class Tok:
    __slots__ = ("w", "r")

    def __init__(self):
        self.w = None
        self.r = {}


class Eng:
    def __init__(self, ctx, eng, name, is_pe=False):
        self.ctx = ctx
        self.eng = eng
        self.name = name
        self.is_pe = is_pe
        self.sem = ctx.es.enter_context(ctx.nc.semaphore("s_" + name))
        self.count = 0
        self.waited = {}

    def wait(self, ev):
        sem, val, src = ev
        if src is self and self.is_pe:
            return
        if self.waited.get(sem, 0) >= val:
            return
        self.eng.wait_ge(sem, val)
        self.waited[sem] = val

    def signal(self, ins):
        self.count += 1
        ins.then_inc(self.sem, 1)
        return (self.sem, self.count, self)


class Ctx:
    NDMA = 24

    def __init__(self, nc):
        self.nc = nc
        self.es = ExitStack()
        self.pe = Eng(self, nc.tensor, "pe", True)
        self.act = Eng(self, nc.scalar, "act")
        self.dve = Eng(self, nc.vector, "dve")
        self.pool = Eng(self, nc.gpsimd, "pool")
        self.sp = Eng(self, nc.sync, "sp")
        self.engs = [self.pe, self.act, self.dve, self.pool, self.sp]
        self.dsem = [[self.es.enter_context(nc.semaphore("d%d" % i)), 0] for i in range(self.NDMA)]
        self.di = {}
        self.uid = 0

    def _deps(self, E, reads, writes):
        for b in reads:
            if b.w is not None:
                E.wait(b.w)
        for b in writes:
            if b.w is not None:
                E.wait(b.w)
            for ev in b.r.values():
                E.wait(ev)

    def _reg(self, ev, reads, writes):
        for b in reads:
            b.r[ev[0]] = ev
        for b in writes:
            b.w = ev
            b.r = {}

    def op(self, E, fn, reads=(), writes=()):
        self._deps(E, reads, writes)
        ins = fn()
        ev = E.signal(ins)
        self._reg(ev, reads, writes)
        return ev

    def dma(self, Q, out_ap, in_ap, reads=(), writes=()):
        self._deps(Q, reads, writes)
        half = self.NDMA // 2
        base = 0 if Q is self.sp else half
        n = self.di.get(base, 0)
        self.di[base] = n + 1
        slot = self.dsem[base + n % half]
        if slot[1] > 0:
            Q.wait((slot[0], 16 * slot[1], None))
        ins = Q.eng.dma_start(out=out_ap, in_=in_ap)
        slot[1] += 1
        ins.then_inc(slot[0], 16)
        ev = (slot[0], 16 * slot[1], None)
        self._reg(ev, reads, writes)
        return ev

    def mm(self, out_ap, pairs, reads=(), writes=(), start=True, stop=True):
        n = len(pairs)

        def fn():
            ins = None
            for i, (l, r) in enumerate(pairs):
                ins = self.nc.tensor.matmul(out_ap, l, r, start=(start and i == 0), stop=(stop and i == n - 1))
            return ins
        return self.op(self.pe, fn, reads, writes)

    def barrier(self):
        evs = [(E.sem, E.count, E) for E in self.engs if E.count > 0]
        evs += [(s[0], 16 * s[1], None) for s in self.dsem if s[1] > 0]
        for E in self.engs:
            for ev in evs:
                if ev[2] is not E:
                    E.wait(ev)

    def name(self, p):
        self.uid += 1
        return "%s_%d" % (p, self.uid)


class Scope:
    def __init__(self, ctx):
        self.ctx = ctx
        self.es = ExitStack()

    def sb(self, shape, dt, name="t"):
        return self.es.enter_context(self.ctx.nc.sbuf_tensor(self.ctx.name(name), list(shape), dt))

    def ps(self, shape=(128, 512), dt=F32, name="p"):
        return self.es.enter_context(self.ctx.nc.psum_tensor(self.ctx.name(name), list(shape), dt))

    def close(self):
        self.es.close()


def build(nlayers=DEPTH, stop=None, debug=False):
    nc = bass.Bass("TRN2", target_bir_lowering=False)

    def din(name, shape, dt=F32):
        return nc.dram_tensor(name, list(shape), dt, kind="ExternalInput").ap()

    def dscr(name, shape, dt):
        return nc.dram_tensor(name, list(shape), dt, kind=("ExternalOutput" if debug else "Internal")).ap()

    x_d = din("x", [S, D])
    w_in_d = din("w_in", [DEPTH, D, PC])
    wa_d = din("wa_bd", [DEPTH, 4, 128, 128])
    wx_d = din("wx_bd", [DEPTH, 4, 128, 128])
    wbr_d = din("w_branch", [DEPTH, 3, 512, D])
    wout_d = din("w_out", [DEPTH, D, D])
    wr_d = din("w_router", [DEPTH, D, 20])
    wg_d = din("w_gate", [DEPTH, 16, D, 512])
    wu_d = din("w_up", [DEPTH, 16, D, 512])
    wd_d = din("w_down", [DEPTH, 16, 512, D])
    vecs_d = din("vecs", [DEPTH, 128, NV])
    lnin_d = din("lnin", [128, 16])
    rb_d = din("rbias", [DEPTH, 128, 20])
    ident_d = din("ident", [128, 128])
    negtri_d = din("negtri", [128, 128])
    masks_d = din("masks", [128, 8 * 512])
    sel_d = din("sel", [16, 16 * 128])
    out_d = nc.dram_tensor("out", [S, D], F32, kind="ExternalOutput").ap()

    h32_d = dscr("h32", [NT, 128, 8, TT], F32)
    hbf_d = dscr("hbf", [NT, 128, 8, TT], BF16)
    ysb_d = dscr("ysb", [512, S], BF16)
    ylru_d = dscr("ylru", [512, S], BF16)
    ysc_d = dscr("ysc", [512, S], BF16)
    mT_d = dscr("mT", [NT, 128, 8, TT], BF16)

    def fm(ap):
        return ap.rearrange("(c p) t -> p c t", p=128)

    C = Ctx(nc)
    pe, act, dve, pool, sp = C.engs

    G = Scope(C)
    ident = G.sb([128, 128], F32, "ident")
    negtri = G.sb([128, 128], BF16, "negtri")
    ones_bf = G.sb([128, 128], BF16, "ones")
    negones = G.sb([128, 128], BF16, "negones")
    mean_bf = G.sb([128, 128], BF16, "meanm")
    masks = G.sb([128, 8 * 512], BF16, "masks")
    sel = G.sb([16, 16 * 128], BF16, "sel")
    lnin = G.sb([128, 16], F32, "lnin")
    vecs = G.sb([128, DEPTH * NV], F32, "vecs")
    rbias = G.sb([128, DEPTH * 20], F32, "rbias")
    cchan = G.sb([128, DEPTH * 4], F32, "cchan")
    wr_sb = G.sb([128, 8, 20], F32, "wr")
    lneps = G.sb([128, 1], F32, "lneps")
    lg = G.sb([128, 32, 20], F32, "lg")
    t_wr, t_lg = Tok(), Tok()
    k_const = Tok()
    C.dma(sp, ident[:], ident_d[:, :], writes=[k_const])
    C.dma(pool, negtri[:], negtri_d[:, :], writes=[k_const])
    C.dma(pool, masks[:], masks_d[:, :], writes=[k_const])
    C.dma(pool, sel[:], sel_d[:, :], writes=[k_const])
    C.dma(sp, lnin[:], lnin_d[:, :], writes=[k_const])
    for l in range(DEPTH):
        C.dma(sp, vecs[:, l * NV:(l + 1) * NV], vecs_d[l], writes=[k_const])
        C.dma(sp, rbias[:, l * 20:(l + 1) * 20], rb_d[l], writes=[k_const])
    C.op(dve, lambda: nc.vector.memset(ones_bf[:], 1.0), writes=[k_const])
    C.op(dve, lambda: nc.vector.memset(mean_bf[:], 1.0 / D), writes=[k_const])
    C.op(dve, lambda: nc.vector.memset(negones[:], -1.0), writes=[k_const])
    C.op(dve, lambda: nc.vector.memset(lneps[:], LN_EPS), writes=[k_const])
    C.barrier()
    for l in range(DEPTH):
        C.op(act, lambda l=l: nc.scalar.activation(out=cchan[:, l * 4:(l + 1) * 4], in_=vecs[:, l * NV + 84:l * NV + 88],
                                                   func=AF.Exp, scale=-1.0), reads=[k_const], writes=[k_const])
        C.op(act, lambda l=l: nc.scalar.activation(out=cchan[:, l * 4:(l + 1) * 4], in_=cchan[:, l * 4:(l + 1) * 4],
                                                   func=AF.Ln, bias=1.0), reads=[k_const], writes=[k_const])
    C.op(dve, lambda: nc.vector.tensor_scalar(out=cchan[:], in0=cchan[:], scalar1=-8.0, scalar2=None, op0=ALU.mult),
         reads=[k_const], writes=[k_const])
    C.barrier()

    def vcol(l, c0, n=1):
        return vecs[:, l * NV + c0:l * NV + c0 + n]

    def ln_alloc(Sc, nsets, final=False):
        L = {"sets": [], "ps": [], "n": nsets}
        for _ in range(nsets):
            W = {}
            W["xb"] = Sc.sb([128, 8, TT], BF16, "lnxb")
            W["sq"] = Sc.sb([128, 8, TT], BF16, "lnsq")
            W["hb"] = Sc.sb([128, 8, TT], BF16, "lnhb")
            W["mean_sb"] = Sc.sb([128, TT], F32, "lnmean")
            W["msq"] = Sc.sb([128, TT], F32, "lnmsq")
            W["rstd"] = Sc.sb([128, TT], F32, "lnrstd")
            for k in ("t_xb", "t_sq", "t_hb", "t_st"):
                W[k] = Tok()
            W["t_c"] = [Tok() for _ in range(8)]
            L["sets"].append(W)
        for _ in range(2):
            L["ps"].append(dict(mean_ps=Sc.ps(name="lnmps"), ex2_ps=Sc.ps(name="lneps"), t_mps=Tok(), t_eps=Tok()))
        if final:
            L["ot"] = [Sc.sb([128, D], F32, "lnot") for _ in range(2)]
            L["t_ot"] = [Tok(), Tok()]
            L["tp_ps"] = [Sc.ps(name="lntp") for _ in range(2)]
            L["t_tp"] = [Tok(), Tok()]
        return L

    def ln_s1(L, k, xs, xs_tok):
        W = L["sets"][k % L["n"]]
        Pp = L["ps"][k % 2]
        xb, sq = W["xb"], W["sq"]
        C.op(dve, lambda: nc.vector.tensor_copy(out=xb[:], in_=xs[:]), reads=[xs_tok], writes=[W["t_xb"]])
        C.op(act, lambda: nc.scalar.activation(out=sq[:], in_=xs[:], func=AF.Square), reads=[xs_tok], writes=[W["t_sq"]])
        C.mm(Pp["mean_ps"][:], [(mean_bf[:], xb[:, c, :]) for c in range(8)], reads=[W["t_xb"]], writes=[Pp["t_mps"]])
        C.mm(Pp["ex2_ps"][:], [(mean_bf[:], sq[:, c, :]) for c in range(8)], reads=[W["t_sq"]], writes=[Pp["t_eps"]])

    def ln_s2(L, k):
        W = L["sets"][k % L["n"]]
        Pp = L["ps"][k % 2]
        mean_sb, msq, rstd, t_st = W["mean_sb"], W["msq"], W["rstd"], W["t_st"]
        C.op(act, lambda: nc.scalar.activation(out=mean_sb[:], in_=Pp["mean_ps"][:], func=AF.Copy), reads=[Pp["t_mps"]], writes=[t_st])
        C.op(act, lambda: nc.scalar.activation(out=msq[:], in_=Pp["mean_ps"][:], func=AF.Square), reads=[Pp["t_mps"]], writes=[t_st])
        C.op(dve, lambda: nc.vector.tensor_tensor(out=rstd[:], in0=Pp["ex2_ps"][:], in1=msq[:], op=ALU.subtract),
             reads=[Pp["t_eps"], t_st], writes=[t_st])
        C.op(act, lambda: nc.scalar.activation(out=rstd[:], in_=rstd[:], func=AF.Ln, bias=lneps[:, 0:1]), reads=[t_st], writes=[t_st])
        C.op(act, lambda: nc.scalar.activation(out=rstd[:], in_=rstd[:], func=AF.Exp, scale=-0.5), reads=[t_st], writes=[t_st])

    def ln_s3(L, k, xs, xs_tok, gcol, bcol, tile_i, final, router=None):
        W = L["sets"][k % L["n"]]
        hb, t_hb, t_st = W["hb"], W["t_hb"], W["t_st"]
        mean_sb, rstd = W["mean_sb"], W["rstd"]
        for c in range(8):
            E = pool if c in (3, 7) else dve
            ee = E.eng
            tk = W["t_c"][c]
            C.op(E, lambda: ee.tensor_tensor(out=xs[:, c, :], in0=xs[:, c, :], in1=mean_sb[:], op=ALU.subtract),
                 reads=[t_st, xs_tok, W["t_xb"], W["t_sq"]], writes=[tk])
            C.op(E, lambda: ee.tensor_tensor(out=xs[:, c, :], in0=xs[:, c, :], in1=rstd[:], op=ALU.mult),
                 reads=[t_st, tk], writes=[tk])
            if not final:
                C.op(act, lambda: nc.scalar.activation(out=hb[:, c, :], in_=xs[:, c, :], func=AF.Identity,
                                                       scale=gcol[:, c:c + 1], bias=bcol[:, c:c + 1]), reads=[tk], writes=[t_hb])
            C.op(act, lambda: nc.scalar.activation(out=xs[:, c, :], in_=xs[:, c, :], func=AF.Identity,
                                                   scale=gcol[:, c:c + 1], bias=bcol[:, c:c + 1]), reads=[tk], writes=[tk])
        tsl = slice(tile_i * TT, (tile_i + 1) * TT)
        if router is not None:
            wr, t_wr, lpsL, t_lpsL, lg, t_lg, lg_copy = router
            if tile_i > 0:
                lg_copy(tile_i - 1)
            lps, t_lps = lpsL[tile_i % 2], t_lpsL[tile_i % 2]
            for tb in range(4):
                C.mm(lps[:, tb * 32:tb * 32 + 20], [(xs[:, c, tb * 128:(tb + 1) * 128], wr[:, c, :]) for c in range(8)],
                     reads=W["t_c"] + [t_wr], writes=[t_lps])
        if not final:
            C.dma(sp, h32_d[tile_i], xs[:], writes=[xs_tok] + W["t_c"])
            C.dma(sp, hbf_d[tile_i], hb[:], reads=[t_hb])
        else:
            ot, t_ot, tp_ps, t_tp = L["ot"], L["t_ot"], L["tp_ps"], L["t_tp"]
            for tb in range(4):
                for half in range(2):
                    kk = (tb * 2 + half) % 2
                    for cc in range(4):
                        c = half * 4 + cc
                        C.mm(tp_ps[kk][:, cc * 128:(cc + 1) * 128], [(xs[:, c, tb * 128:(tb + 1) * 128], ident[:])],
                             reads=[W["t_c"][c], xs_tok], writes=[t_tp[kk]])
                    C.op(act, lambda: nc.scalar.activation(out=ot[tb % 2][:, half * 512:(half + 1) * 512], in_=tp_ps[kk][:], func=AF.Copy),
                         reads=[t_tp[kk]], writes=[t_ot[tb % 2]])
                r0 = tile_i * TT + tb * 128
                C.dma(sp, out_d[r0:r0 + 128, :], ot[tb % 2][:], reads=[t_ot[tb % 2]])

    def pipeline(n, stages, reverse=False):
        ns = len(stages)
        order = list(enumerate(stages))
        if reverse:
            order = order[::-1]
        for step in range(n + ns - 1):
            for si, st in order:
                if 0 <= step - si < n:
                    st(step - si)

    def phase0():
        Sc = Scope(C)
        L = ln_alloc(Sc, 2)
        xt = [Sc.sb([128, 4, D], F32, "xt") for _ in range(2)]
        t_xt = [Tok(), Tok()]
        xs = [Sc.sb([128, 8, TT], F32, "xs") for _ in range(3)]
        t_xs = [Tok() for _ in range(3)]
        tp = [Sc.ps(name="tp") for _ in range(2)]
        t_tp = [Tok(), Tok()]
        xv = x_d.rearrange("(n tb p) d -> n p tb d", tb=4, p=128)

        def T0(i):
            C.dma(sp, xt[i % 2][:], xv[i], writes=[t_xt[i % 2]])

        def T1(i):
            b = i % 2
            x3 = i % 3
            for c in range(8):
                k = c % 2
                for tb in range(4):
                    C.mm(tp[k][:, tb * 128:(tb + 1) * 128], [(xt[b][:, tb, c * 128:(c + 1) * 128], ident[:])],
                         reads=[t_xt[b]], writes=[t_tp[k]])
                if c % 2 == 0:
                    C.op(act, lambda: nc.scalar.activation(out=xs[x3][:, c, :], in_=tp[k][:], func=AF.Copy),
                         reads=[t_tp[k]], writes=[t_xs[x3]])
                else:
                    C.op(dve, lambda: nc.vector.tensor_copy(out=xs[x3][:, c, :], in_=tp[k][:]), reads=[t_tp[k]], writes=[t_xs[x3]])
            ln_s1(L, i, xs[x3], t_xs[x3])

        def T2(i):
            ln_s2(L, i)

        def T3(i):
            ln_s3(L, i, xs[i % 3], t_xs[i % 3], lnin[:, 0:8], lnin[:, 8:16], i, False)
        pipeline(NT, [T0, T1, T2, T3], reverse=True)
        C.barrier()
        Sc.close()

    def load_w_cols(dst, tok, l, col0, ncols):
        src = w_in_d[l].rearrange("(c p) e -> p c e", p=128)[:, :, col0:col0 + ncols]
        C.dma(pool, dst[:], src, writes=[tok])

    def load_hbf(Sc):
        hb = Sc.sb([128, 8, S], BF16, "hbres")
        toks = [Tok() for _ in range(NT)]
        for i in range(NT):
            C.dma(sp, hb[:, :, i * TT:(i + 1) * TT], hbf_d[i], writes=[toks[i]])
        return hb, toks

    def phaseA(l, hb, hb_t):
        Sc = Scope(C)
        wq = [Sc.sb([128, 8, 384], BF16, "wqkv") for _ in range(2)]
        t_wq = [Tok(), Tok()]
        qT2 = [Sc.sb([128, S], BF16, "qT") for _ in range(2)]
        kT2 = [Sc.sb([128, S], BF16, "kT") for _ in range(2)]
        vv2 = [Sc.sb([128, 32, 128], BF16, "vv") for _ in range(2)]
        t_q2 = [[Tok() for _ in range(NT)] for _ in range(2)]
        t_k2 = [[Tok() for _ in range(NT)] for _ in range(2)]
        t_v2 = [[Tok() for _ in range(NT)] for _ in range(2)]
        W2 = 2 * TT
        NU, NSP, NE, NW = 2, 5, 3, 4
        u_sb = [Sc.sb([128, W2], F32, "u") for _ in range(NU)]
        sp_sb = [Sc.sb([128, W2], BF16, "spl") for _ in range(NSP)]
        e_sb = [Sc.sb([128, W2], F32, "e") for _ in range(NE)]
        w_sb = [Sc.sb([128, W2], BF16, "w") for _ in range(NW)]
        carry = [Sc.sb([128, W2], F32, "carry") for _ in range(2)]
        yo = [Sc.sb([128, TT], BF16, "yo") for _ in range(2)]
        t_u = [Tok() for _ in range(NU)]
        t_sp = [Tok() for _ in range(NSP)]
        t_e = [Tok() for _ in range(NE)]
        t_w = [Tok() for _ in range(NW)]
        t_carry = [Tok(), Tok()]
        t_yo = [Tok(), Tok()]
        _z = Sc.ps([128, W2], name="z")
        z_ps = [_z, _z]
        a_ps = Sc.ps([128, W2], name="a")
        c_ps = Sc.ps([128, W2], name="c")
        pv_ps = Sc.ps(name="pv")
        pj_ps = Sc.ps(name="pj")
        t_pj = Tok()
        _tz = Tok()
        t_z = [_tz, _tz]
        t_a = Tok()
        t_c = Tok()
        t_pv = Tok()

        def load_wq(p):
            for j in range(3):
                src = w_in_d[l].rearrange("(c p) e -> p c e", p=128)[:, :, j * 512 + p * 128: j * 512 + (p + 1) * 128]
                C.dma(pool, wq[p % 2][:, :, j * 128:(j + 1) * 128], src, writes=[t_wq[p % 2]])

        def proj_units(p):
            wb = p % 2
            qd, kd, vd = qT2[wb], kT2[wb], vv2[wb]
            units = []
            for i in range(NT):
                tsl = slice(i * TT, (i + 1) * TT)

                def uq(i=i, tsl=tsl):
                    C.mm(pj_ps[:], [(wq[wb][:, c, 0:128], hb[:, c, tsl]) for c in range(8)],
                         reads=[t_wq[wb], hb_t[i]], writes=[t_pj])
                    C.op(dve, lambda: nc.vector.tensor_scalar(out=qd[:, tsl], in0=pj_ps[:], scalar1=0.125, scalar2=None, op0=ALU.mult),
                         reads=[t_pj], writes=[t_q2[wb][i]])

                def uk(i=i, tsl=tsl):
                    C.mm(pj_ps[:], [(wq[wb][:, c, 128:256], hb[:, c, tsl]) for c in range(8)],
                         reads=[t_wq[wb], hb_t[i]], writes=[t_pj])
                    C.op(dve, lambda: nc.vector.tensor_copy(out=kd[:, tsl], in_=pj_ps[:]), reads=[t_pj], writes=[t_k2[wb][i]])

                def uv(i=i):
                    for tb in range(4):
                        blk = i * 4 + tb
                        C.mm(pj_ps[:, tb * 128:(tb + 1) * 128],
                             [(hb[:, c, blk * 128:(blk + 1) * 128], wq[wb][:, c, 256:384]) for c in range(8)],
                             reads=[t_wq[wb], hb_t[i]], writes=[t_pj])
                    C.op(dve, lambda: nc.vector.tensor_copy(out=vd[:, i * 4:(i + 1) * 4, :],
                                                            in_=pj_ps[:].rearrange("p (a b) -> p a b", b=128)),
                         reads=[t_pj], writes=[t_v2[wb][i]])
                units += [uq, uk, uv]
            return units

        load_wq(0)
        for u in proj_units(0):
            u()
        for p in range(4):
            wb = p % 2
            qT, kT, vv = qT2[wb], kT2[wb], vv2[wb]
            t_q, t_k, t_v = t_q2[wb], t_k2[wb], t_v2[wb]
            nxt = []
            if p + 1 < 4:
                load_wq(p + 1)
                nxt = proj_units(p + 1)
            steps = []
            sweep = 0
            for i in range(NT):
                nk = 4 * (i + 1)
                for m in range(nk):
                    kb = nk - 1 - m
                    steps.append(dict(i=i, kb=kb, first=(m == 0), last=(m == nk - 1), dj=(kb - 4 * i if kb >= 4 * i else None), sw=sweep))
                sweep += 1
            NST = len(steps)
            h0, h1 = slice(0, 64), slice(64, 128)

            def S1(n):
                T = steps[n]
                tsl = slice(T["i"] * TT, (T["i"] + 1) * TT)
                ks = slice(T["kb"] * 128, (T["kb"] + 1) * 128)
                ru, r = n % NU, n % NSP

                def fz():
                    nc.tensor.matmul(z_ps[0][:, 0:TT], kT[h0, ks], qT[h0, tsl], start=True, stop=True)
                    return nc.tensor.matmul(z_ps[0][:, TT:W2], kT[h1, ks], qT[h1, tsl], start=True, stop=True)
                C.op(pe, fz, reads=[t_k[T["kb"] // 4], t_q[T["i"]]], writes=[t_z[0]])
                C.op(act, lambda: nc.scalar.activation(out=u_sb[ru][:], in_=z_ps[0][:], func=AF.Exp), reads=[t_z[0]], writes=[t_u[ru]])
                C.op(act, lambda: nc.scalar.activation(out=sp_sb[r][:], in_=u_sb[ru][:], func=AF.Ln, bias=1.0),
                     reads=[t_u[ru]], writes=[t_sp[r]])
                if T["dj"] is not None:
                    m = masks[:, T["dj"] * W2:(T["dj"] + 1) * W2]
                    C.op(dve, lambda: nc.vector.tensor_tensor(out=sp_sb[r][:], in0=sp_sb[r][:], in1=m, op=ALU.mult),
                         reads=[t_sp[r]], writes=[t_sp[r]])

            def S2(n):
                T = steps[n]
                tsl = slice(T["i"] * TT, (T["i"] + 1) * TT)
                ks = slice(T["kb"] * 128, (T["kb"] + 1) * 128)
                r, re, rw = n % NSP, n % NE, n % NW
                cy = carry[T["sw"] % 2]
                t_cy = t_carry[T["sw"] % 2]
                sp1 = sp_sb[r][:, 0:TT]
                sp2 = sp_sb[r][:, TT:W2]

                def fa():
                    nc.tensor.matmul(a_ps[:, 0:TT], kT[h0, ks], qT[h0, tsl], start=True, stop=False)
                    nc.tensor.matmul(a_ps[:, TT:W2], kT[h1, ks], qT[h1, tsl], start=True, stop=False)
                    nc.tensor.matmul(a_ps[:, 0:TT], negtri[:], sp1, start=False, stop=True)
                    return nc.tensor.matmul(a_ps[:, TT:W2], negtri[:], sp2, start=False, stop=True)
                C.op(pe, fa, reads=[t_sp[r], t_k[T["kb"] // 4], t_q[T["i"]]], writes=[t_a])
                if not T["last"]:
                    def fc():
                        nc.tensor.matmul(c_ps[:, 0:TT], ones_bf[:], sp1, start=True, stop=True)
                        return nc.tensor.matmul(c_ps[:, TT:W2], ones_bf[:], sp2, start=True, stop=True)
                    C.op(pe, fc, reads=[t_sp[r]], writes=[t_c])
                if T["first"]:
                    C.op(dve, lambda: nc.vector.tensor_copy(out=e_sb[re][:], in_=a_ps[:]), reads=[t_a], writes=[t_e[re]])
                    if not T["last"]:
                        C.op(dve, lambda: nc.vector.tensor_scalar(out=cy[:], in0=c_ps[:], scalar1=-1.0, scalar2=None, op0=ALU.mult),
                             reads=[t_c], writes=[t_cy])
                else:
                    C.op(dve, lambda: nc.vector.tensor_tensor(out=e_sb[re][:], in0=a_ps[:], in1=cy[:], op=ALU.add),
                         reads=[t_a, t_cy], writes=[t_e[re]])
                    if not T["last"]:
                        C.op(dve, lambda: nc.vector.tensor_tensor(out=cy[:], in0=cy[:], in1=c_ps[:], op=ALU.subtract),
                             reads=[t_c, t_cy], writes=[t_cy])
                C.op(act, lambda: nc.scalar.activation(out=w_sb[rw][:], in_=e_sb[re][:], func=AF.Exp), reads=[t_e[re]], writes=[t_w[rw]])
                if T["dj"] is not None:
                    m = masks[:, T["dj"] * W2:(T["dj"] + 1) * W2]
                    C.op(pool, lambda: nc.gpsimd.tensor_tensor(out=w_sb[rw][:], in0=w_sb[rw][:], in1=m, op=ALU.mult),
                         reads=[t_w[rw]], writes=[t_w[rw]])

            def S3(n):
                T = steps[n]
                rw = n % NW
                pb = T["sw"] % 2

                def fp():
                    nc.tensor.matmul(pv_ps[0:64, :], vv[:, T["kb"], 0:64], w_sb[rw][:, 0:TT], start=T["first"], stop=T["last"])
                    return nc.tensor.matmul(pv_ps[64:128, :], vv[:, T["kb"], 64:128], w_sb[rw][:, TT:W2], start=T["first"], stop=T["last"])
                C.op(pe, fp, reads=[t_w[rw], t_v[T["kb"] // 4], t_pv], writes=[t_pv])
                if T["last"]:
                    C.op(dve, lambda: nc.vector.tensor_copy(out=yo[pb][:], in_=pv_ps[:]), reads=[t_pv], writes=[t_yo[pb]])
                    C.dma(sp, ysb_d[p * 128:(p + 1) * 128, T["i"] * TT:(T["i"] + 1) * TT], yo[pb][:], reads=[t_yo[pb]])

            for n in range(NST + 5):
                if n < NST:
                    S1(n)
                if 0 <= n - 3 < NST:
                    S2(n - 3)
                if 0 <= n - 5 < NST:
                    S3(n - 5)
                if nxt and n % 5 == 4:
                    nxt.pop(0)()
            while nxt:
                nxt.pop(0)()
        C.barrier()
        Sc.close()

    def phaseB(l, hb, hb_t):
        Sc = Scope(C)
        wl = [Sc.sb([128, 8, 256], BF16, "wl") for _ in range(2)]
        t_wl = [Tok(), Tok()]
        wab = [Sc.sb([128, 256], BF16, "wab") for _ in range(2)]
        t_wab = [Tok(), Tok()]
        lin = Sc.sb([128, 3 + S], F32, "lin")
        ub = Sc.sb([128, S], F32, "ub")
        rb_ = Sc.sb([128, S], F32, "rb")
        ib = Sc.sb([128, S], F32, "ib")
        ubf = Sc.sb([128, S], BF16, "ubf")
        yb = Sc.sb([128, S], BF16, "yb")
        NH = 2
        HS = S // NH
        t_lin = Tok()
        t_ub = [Tok() for _ in range(NH)]
        t_rb = [Tok() for _ in range(NH)]
        t_ib = [Tok() for _ in range(NH)]
        t_ubf = [Tok() for _ in range(NH)]
        t_yb = [Tok() for _ in range(NH)]
        ps = [Sc.ps(name="bps") for _ in range(4)]
        t_ps = [Tok() for _ in range(4)]
        C.op(dve, lambda: nc.vector.memset(lin[:, 0:3], 0.0), writes=[t_lin])
        for c in range(4):
            wbuf = c % 2
            src = w_in_d[l].rearrange("(c p) e -> p c e", p=128)
            C.dma(pool, wl[wbuf][:, :, 0:128], src[:, :, 1536 + c * 128:1536 + (c + 1) * 128], writes=[t_wl[wbuf]])
            C.dma(pool, wl[wbuf][:, :, 128:256], src[:, :, 2048 + c * 128:2048 + (c + 1) * 128], writes=[t_wl[wbuf]])
            C.dma(pool, wab[wbuf][:, 0:128], wa_d[l, c], writes=[t_wab[wbuf]])
            C.dma(pool, wab[wbuf][:, 128:256], wx_d[l, c], writes=[t_wab[wbuf]])
            for i in range(NT):
                tsl = slice(i * TT, (i + 1) * TT)
                k = i % 4
                C.mm(ps[k][:], [(wl[wbuf][:, cc, 0:128], hb[:, cc, tsl]) for cc in range(8)],
                     reads=[t_wl[wbuf], hb_t[i]], writes=[t_ps[k]])
                C.op(act, lambda: nc.scalar.activation(out=lin[:, 3 + i * TT:3 + (i + 1) * TT], in_=ps[k][:], func=AF.Copy),
                     reads=[t_ps[k]], writes=[t_lin])
            cw = lambda k: vcol(l, 56 + c * 4 + k)
            cc_ap = cchan[:, l * 4 + c:l * 4 + c + 1]
            for h in range(NH):
                hs = slice(h * HS, (h + 1) * HS)
                C.op(dve, lambda: nc.vector.tensor_scalar(out=ub[:, hs], in0=lin[:, h * HS:h * HS + HS], scalar1=cw(0), scalar2=vcol(l, 72 + c),
                                                          op0=ALU.mult, op1=ALU.add), reads=[t_lin], writes=[t_ub[h]])
                for k in range(1, 4):
                    C.op(dve, lambda: nc.vector.scalar_tensor_tensor(out=ub[:, hs], in0=lin[:, h * HS + k:h * HS + k + HS], scalar=cw(k),
                                                                     in1=ub[:, hs], op0=ALU.mult, op1=ALU.add),
                         reads=[t_lin, t_ub[h]], writes=[t_ub[h]])
                C.op(act, lambda: nc.scalar.activation(out=ubf[:, hs], in_=ub[:, hs], func=AF.Copy), reads=[t_ub[h]], writes=[t_ubf[h]])
            for h in range(NH):
                for i in range(h * (NT // NH), (h + 1) * (NT // NH)):
                    tsl = slice(i * TT, (i + 1) * TT)
                    k = i % 2
                    C.mm(ps[k][:], [(wab[wbuf][:, 0:128], ubf[:, tsl])], reads=[t_wab[wbuf], t_ubf[h]], writes=[t_ps[k]])
                    C.op(act, lambda: nc.scalar.activation(out=rb_[:, tsl], in_=ps[k][:], func=AF.Sigmoid, bias=vcol(l, 76 + c)),
                         reads=[t_ps[k]], writes=[t_rb[h]])
                    C.mm(ps[2 + k][:], [(wab[wbuf][:, 128:256], ubf[:, tsl])], reads=[t_wab[wbuf], t_ubf[h]], writes=[t_ps[2 + k]])
                    C.op(act, lambda: nc.scalar.activation(out=ib[:, tsl], in_=ps[2 + k][:], func=AF.Sigmoid, bias=vcol(l, 80 + c)),
                         reads=[t_ps[2 + k]], writes=[t_ib[h]])
            for h in range(NH):
                hs = slice(h * HS, (h + 1) * HS)
                C.op(act, lambda: nc.scalar.activation(out=rb_[:, hs], in_=rb_[:, hs], func=AF.Exp, scale=cc_ap),
                     reads=[t_rb[h]], writes=[t_rb[h]])
            for h in range(NH):
                hs = slice(h * HS, (h + 1) * HS)
                C.op(dve, lambda: nc.vector.tensor_tensor(out=ib[:, hs], in0=ib[:, hs], in1=ub[:, hs], op=ALU.mult),
                     reads=[t_ib[h], t_ub[h]], writes=[t_ib[h]])
                C.op(dve, lambda: nc.vector.tensor_tensor(out=ub[:, hs], in0=rb_[:, hs], in1=rb_[:, hs], op=ALU.mult),
                     reads=[t_rb[h], t_ib[h], t_ubf[h]], writes=[t_ub[h]])
                C.op(dve, lambda: nc.vector.tensor_scalar(out=ub[:, hs], in0=ub[:, hs], scalar1=-1.0, scalar2=1.0, op0=ALU.mult, op1=ALU.add),
                     reads=[t_ub[h]], writes=[t_ub[h]])
            for h in range(NH):
                hs = slice(h * HS, (h + 1) * HS)
                C.op(act, lambda: nc.scalar.activation(out=ub[:, hs], in_=ub[:, hs], func=AF.Sqrt), reads=[t_ub[h]], writes=[t_ub[h]])
            for h in range(NH):
                hs = slice(h * HS, (h + 1) * HS)
                C.op(dve, lambda: nc.vector.tensor_tensor(out=ib[:, hs], in0=ib[:, hs], in1=ub[:, hs], op=ALU.mult),
                     reads=[t_ib[h], t_ub[h]], writes=[t_ib[h]])
                init = 0.0 if h == 0 else ub[:, h * HS - 1:h * HS]
                rd = [t_rb[h], t_ib[h], t_ub[h]] + ([t_ub[h - 1]] if h > 0 else [])
                C.op(dve, lambda: nc.vector.tensor_tensor_scan(out=ub[:, hs], data0=rb_[:, hs], data1=ib[:, hs], initial=init,
                                                               op0=ALU.mult, op1=ALU.add), reads=rd, writes=[t_ub[h]])
            for h in range(NH):
                hs = slice(h * HS, (h + 1) * HS)
                for i in range(h * (NT // NH), (h + 1) * (NT // NH)):
                    tsl = slice(i * TT, (i + 1) * TT)
                    k = i % 4
                    C.mm(ps[k][:], [(wl[wbuf][:, cc, 128:256], hb[:, cc, tsl]) for cc in range(8)],
                         reads=[t_wl[wbuf], hb_t[i]], writes=[t_ps[k]])
                    C.op(act, lambda: nc.scalar.activation(out=rb_[:, tsl], in_=ps[k][:], func=AF.Gelu_apprx_tanh),
                         reads=[t_ps[k], t_ub[h]], writes=[t_rb[h]])
                C.op(dve, lambda: nc.vector.tensor_tensor(out=yb[:, hs], in0=rb_[:, hs], in1=ub[:, hs], op=ALU.mult),
                     reads=[t_rb[h], t_ub[h]], writes=[t_yb[h]])
                C.dma(sp, ylru_d[c * 128:(c + 1) * 128, hs], yb[:, hs], reads=[t_yb[h]])
        C.barrier()
        Sc.close()

    def phaseC(l, hb, hb_t):
        Sc = Scope(C)
        wl = [Sc.sb([128, 8, 384], BF16, "wc") for _ in range(2)]
        t_wl = [Tok(), Tok()]
        cb = [Sc.sb([128, S], F32, "ccb") for _ in range(2)]
        ph = [Sc.sb([128, 2 + S], F32, "cph") for _ in range(2)]
        vb = Sc.sb([128, S], F32, "cvb")
        yb = Sc.sb([128, S], BF16, "cyb")
        t_cb, t_ph = [Tok(), Tok()], [Tok(), Tok()]
        t_vb, t_yb = Tok(), Tok()
        ps = [Sc.ps(name="cps") for _ in range(4)]
        t_ps = [Tok() for _ in range(4)]
        for b in range(2):
            C.op(dve, lambda: nc.vector.memset(ph[b][:, 0:2], 0.0), writes=[t_ph[b]])
        src = w_in_d[l].rearrange("(c p) e -> p c e", p=128)

        def front(c):
            wbuf = c % 2
            for j in range(3):
                C.dma(pool, wl[wbuf][:, :, j * 128:(j + 1) * 128], src[:, :, 2560 + j * 512 + c * 128:2560 + j * 512 + (c + 1) * 128],
                      writes=[t_wl[wbuf]])
            for i in range(NT):
                tsl = slice(i * TT, (i + 1) * TT)
                k = i % 2
                C.mm(ps[k][:], [(wl[wbuf][:, cc, 128:256], hb[:, cc, tsl]) for cc in range(8)],
                     reads=[t_wl[wbuf], hb_t[i]], writes=[t_ps[k]])
                C.op(act, lambda: nc.scalar.activation(out=cb[wbuf][:, tsl], in_=ps[k][:], func=AF.Copy), reads=[t_ps[k]], writes=[t_cb[wbuf]])
                C.mm(ps[2 + k][:], [(wl[wbuf][:, cc, 256:384], hb[:, cc, tsl]) for cc in range(8)],
                     reads=[t_wl[wbuf], hb_t[i]], writes=[t_ps[2 + k]])
                C.op(dve, lambda: nc.vector.tensor_tensor(out=ph[wbuf][:, 2 + i * TT:2 + (i + 1) * TT], in0=ps[2 + k][:], in1=cb[wbuf][:, tsl],
                                                          op=ALU.mult), reads=[t_ps[2 + k], t_cb[wbuf]], writes=[t_ph[wbuf]])

        def back(c):
            wbuf = c % 2
            cw = lambda k: vcol(l, 88 + c * 3 + k)
            C.op(dve, lambda: nc.vector.tensor_scalar(out=vb[:], in0=ph[wbuf][:, 0:S], scalar1=cw(0), scalar2=None, op0=ALU.mult),
                 reads=[t_ph[wbuf], t_yb], writes=[t_vb])
            for k in range(1, 3):
                C.op(dve, lambda: nc.vector.scalar_tensor_tensor(out=vb[:], in0=ph[wbuf][:, k:k + S], scalar=cw(k), in1=vb[:],
                                                                 op0=ALU.mult, op1=ALU.add), reads=[t_ph[wbuf], t_vb], writes=[t_vb])
            for i in range(NT):
                tsl = slice(i * TT, (i + 1) * TT)
                k = i % 2
                C.mm(ps[k][:], [(wl[wbuf][:, cc, 0:128], hb[:, cc, tsl]) for cc in range(8)],
                     reads=[t_wl[wbuf], hb_t[i]], writes=[t_ps[k]])
                C.op(dve, lambda: nc.vector.tensor_tensor(out=yb[:, tsl], in0=ps[k][:], in1=vb[:, tsl], op=ALU.mult),
                     reads=[t_ps[k], t_vb], writes=[t_yb])
            C.dma(sp, ysc_d[c * 128:(c + 1) * 128, :], yb[:], reads=[t_yb])

        front(0)
        for c in range(4):
            if c + 1 < 4:
                front(c + 1)
            back(c)
        C.barrier()
        Sc.close()

    def phaseD1(l, hb, hb_t):
        Sc = Scope(C)
        wg = Sc.sb([128, 8, 3072], BF16, "wgate")
        wbr = Sc.sb([128, 12, D], BF16, "wbr")
        t_wgs = [Tok() for _ in range(6)]
        t_wbrs = [Tok() for _ in range(3)]
        src = w_in_d[l].rearrange("(c p) e -> p c e", p=128)

        def ld_wg(j):
            C.dma(pool, wg[:, :, j * 512:(j + 1) * 512], src[:, :, 4096 + j * 512:4096 + (j + 1) * 512], writes=[t_wgs[j]])

        def ld_wbr(br):
            C.dma(pool, wbr[:, br * 4:(br + 1) * 4, :], wbr_d[l, br].rearrange("(c p) d -> p c d", p=128), writes=[t_wbrs[br]])
        for br in range(3):
            ld_wg(2 * br)
            ld_wbr(br)
        for br in range(3):
            ld_wg(2 * br + 1)
        yt = [Sc.sb([128, 12, TT], BF16, "yt") for _ in range(2)]
        t_yt = [Tok(), Tok()]
        mt = [Sc.sb([128, 8, TT], BF16, "mt")] * 2
        _tm = Tok()
        t_mt = [_tm, _tm]
        sg = [Sc.sb([128, TT], F32, "sg") for _ in range(3)]
        t_sg = [Tok() for _ in range(3)]
        acc = [Sc.sb([128, TT], F32, "macc") for _ in range(2)]
        t_acc = [Tok(), Tok()]
        tmp = [Sc.sb([128, TT], F32, "mtmp") for _ in range(2)]
        t_tmp = [Tok(), Tok()]
        gps = [Sc.ps(name="gps") for _ in range(3)]
        pps = [Sc.ps(name="pps") for _ in range(3)]
        t_g = [Tok() for _ in range(3)]
        t_p = [Tok() for _ in range(3)]
        ysrc = [ysb_d, ylru_d, ysc_d]
        def load_y(i):
            for br in range(3):
                C.dma(sp, yt[i % 2][:, br * 4:(br + 1) * 4, :],
                      ysrc[br].rearrange("(c p) t -> p c t", p=128)[:, :, i * TT:(i + 1) * TT], writes=[t_yt[i % 2]])
        load_y(0)
        for i in range(NT):
            tsl = slice(i * TT, (i + 1) * TT)
            b = i % 2
            if i + 1 < NT:
                load_y(i + 1)
            for dc in range(8):
                a = dc % 2
                for br in range(3):
                    col = br * 1024 + dc * 128
                    C.mm(gps[br][:], [(wg[:, cc, col:col + 128], hb[:, cc, tsl]) for cc in range(8)],
                         reads=[t_wgs[col // 512], hb_t[i]], writes=[t_g[br]])
                    C.op(act, lambda: nc.scalar.activation(out=sg[br][:], in_=gps[br][:], func=AF.Sigmoid, bias=vcol(l, 32 + br * 8 + dc)),
                         reads=[t_g[br]], writes=[t_sg[br]])
                    C.mm(pps[br][:], [(wbr[:, br * 4 + wc, dc * 128:(dc + 1) * 128], yt[b][:, br * 4 + wc, :]) for wc in range(4)],
                         reads=[t_wbrs[br], t_yt[b]], writes=[t_p[br]])
                C.op(dve, lambda: nc.vector.tensor_tensor(out=acc[a][:], in0=pps[0][:], in1=sg[0][:], op=ALU.mult),
                     reads=[t_p[0], t_sg[0]], writes=[t_acc[a]])
                C.op(dve, lambda: nc.vector.tensor_tensor(out=tmp[0][:], in0=pps[1][:], in1=sg[1][:], op=ALU.mult),
                     reads=[t_p[1], t_sg[1]], writes=[t_tmp[0]])
                C.op(dve, lambda: nc.vector.tensor_tensor(out=tmp[1][:], in0=pps[2][:], in1=sg[2][:], op=ALU.mult),
                     reads=[t_p[2], t_sg[2]], writes=[t_tmp[1]])
                C.op(pool, lambda: nc.gpsimd.tensor_tensor(out=acc[a][:], in0=acc[a][:], in1=tmp[0][:], op=ALU.add),
                     reads=[t_tmp[0], t_acc[a]], writes=[t_acc[a]])
                C.op(pool, lambda: nc.gpsimd.tensor_tensor(out=mt[b][:, dc, :], in0=acc[a][:], in1=tmp[1][:], op=ALU.add),
                     reads=[t_tmp[1], t_acc[a]], writes=[t_mt[b]])
            C.dma(sp, mT_d[i], mt[b][:], reads=[t_mt[b]])
        C.barrier()
        Sc.close()

    def phaseD2(l):
        Sc = Scope(C)
        L = ln_alloc(Sc, 2)
        lps = [Sc.ps(name="lps") for _ in range(2)]
        t_lps = [Tok(), Tok()]

        def lg_copy(i):
            C.op(dve, lambda: nc.vector.tensor_copy(out=lg[:, i * 4:(i + 1) * 4, :],
                                                    in_=lps[i % 2][:, 0:128].rearrange("p (a b) -> p a b", b=32)[:, :, 0:20]),
                 reads=[t_lps[i % 2]], writes=[t_lg])
        C.dma(sp, wr_sb[:], wr_d[l].rearrange("(c p) e -> p c e", p=128), writes=[t_wr])
        wo = Sc.sb([128, 8, D], BF16, "wo")
        t_wo = Tok()
        C.dma(pool, wo[:], wout_d[l].rearrange("(c p) e -> p c e", p=128), writes=[t_wo])
        NX = 4
        mt = [Sc.sb([128, 8, TT], BF16, "mt2") for _ in range(3)]
        t_mt = [Tok() for _ in range(3)]
        xs = [Sc.sb([128, 8, TT], F32, "xs2") for _ in range(NX)]
        t_xs = [Tok() for _ in range(NX)]
        ops_ = [Sc.ps(name="ops") for _ in range(2)]
        t_o = [Tok(), Tok()]

        def T0(i):
            C.dma(sp, mt[i % 3][:], mT_d[i], writes=[t_mt[i % 3]])
            C.dma(sp, xs[i % NX][:], h32_d[i], writes=[t_xs[i % NX]])

        def T1(i):
            b = i % 3
            x3 = i % NX
            for ec in range(8):
                k = ec % 2
                C.mm(ops_[k][:], [(wo[:, dc, ec * 128:(ec + 1) * 128], mt[b][:, dc, :]) for dc in range(8)],
                     reads=[t_wo, t_mt[b]], writes=[t_o[k]])
                C.op(dve, lambda: nc.vector.scalar_tensor_tensor(out=xs[x3][:, ec, :], in0=xs[x3][:, ec, :], scalar=ALPHA, in1=ops_[k][:],
                                                                 op0=ALU.mult, op1=ALU.add), reads=[t_o[k], t_xs[x3]], writes=[t_xs[x3]])
            ln_s1(L, i, xs[x3], t_xs[x3])

        def T2(i):
            ln_s2(L, i)

        def T3(i):
            ln_s3(L, i, xs[i % NX], t_xs[i % NX], vcol(l, 0, 8), vcol(l, 8, 8), i, False,
                  router=(wr_sb, t_wr, lps, t_lps, lg, t_lg, lg_copy))
        pipeline(NT, [T0, T1, T2, T3], reverse=True)
        lg_copy(NT - 1)
        C.barrier()
        Sc.close()

    def phaseE(l, final):
        Sm = Scope(C)
        cT = Sm.sb([16, S], BF16, "cT")
        t_cT = Tok()
        ST = 2048
        oacc = Sm.sb([128, 8, ST], F32, "oacc")
        Sr = Scope(C)
        lps = [Sr.ps(name="lps") for _ in range(2)]
        t_lps = [Tok(), Tok()]
        NB = 32
        t_r = Tok()

        def rt(shape, name):
            return Sr.sb(shape, F32, name)
        glb = rt([128, NB, 4], "glb")
        gmx = rt([128, NB], "gmx")
        ohg = rt([128, NB, 4], "ohg")
        eg = rt([128, NB, 4], "eg")
        gs = rt([128, NB], "gs")
        psel = rt([128, NB], "psel")
        t44 = rt([128, NB, 4, 4], "t44")
        ein = rt([128, NB, 4], "ein")
        sc = rt([128, NB, 4], "sc")
        m1 = rt([128, NB], "m1")
        oh1 = rt([128, NB, 4], "oh1")
        oh2 = rt([128, NB, 4], "oh2")
        l1 = rt([128, NB], "l1")
        l2 = rt([128, NB], "l2")
        w1 = rt([128, NB], "w1")
        w2 = rt([128, NB], "w2")
        cw = rt([128, NB, 4], "cw")
        comb = rt([128, NB, 4, 4], "comb")
        gl = lg[:, :, 0:4]
        el4 = lg[:, :, 4:20].rearrange("p b (g e) -> p b g e", e=4)
        gbias = rbias[:, l * 20:l * 20 + 4]
        ebias = rbias[:, l * 20 + 4:l * 20 + 20].rearrange("p (g e) -> p g e", e=4)

        def bc3(ap2):
            return ap2.unsqueeze(2).to_broadcast([128, NB, 4])

        def V(fn, reads=(t_lg,)):
            C.op(dve, fn, reads=list(reads) + [t_r], writes=[t_r])

        V(lambda: nc.vector.tensor_tensor(out=glb[:], in0=gl, in1=gbias.unsqueeze(1).to_broadcast([128, NB, 4]), op=ALU.add))
        V(lambda: nc.vector.tensor_reduce(out=gmx[:], in_=glb[:], axis=AX.X, op=ALU.max))
        V(lambda: nc.vector.tensor_tensor(out=ohg[:], in0=glb[:], in1=bc3(gmx[:]), op=ALU.is_equal))
        V(lambda: nc.vector.tensor_reduce(out=gmx[:], in_=gl, axis=AX.X, op=ALU.max))
        V(lambda: nc.vector.tensor_tensor(out=eg[:], in0=gl, in1=bc3(gmx[:]), op=ALU.subtract))
        C.op(act, lambda: nc.scalar.activation(out=eg[:], in_=eg[:], func=AF.Exp), reads=[t_r], writes=[t_r])
        V(lambda: nc.vector.tensor_reduce(out=gs[:], in_=eg[:], axis=AX.X, op=ALU.add))
        V(lambda: nc.vector.tensor_tensor(out=eg[:], in0=eg[:], in1=ohg[:], op=ALU.mult))
        V(lambda: nc.vector.tensor_reduce(out=psel[:], in_=eg[:], axis=AX.X, op=ALU.add))
        V(lambda: nc.vector.reciprocal(out=gs[:], in_=gs[:]))
        V(lambda: nc.vector.tensor_tensor(out=psel[:], in0=psel[:], in1=gs[:], op=ALU.mult))
        ohg4 = ohg[:].unsqueeze(3).to_broadcast([128, NB, 4, 4])
        V(lambda: nc.vector.tensor_tensor(out=t44[:], in0=el4, in1=ohg4, op=ALU.mult))
        V(lambda: nc.vector.tensor_reduce(out=ein[:], in_=t44[:].rearrange("p b g e -> p b e g"), axis=AX.X, op=ALU.add))
        V(lambda: nc.vector.tensor_tensor(out=t44[:], in0=ohg4, in1=ebias.unsqueeze(1).to_broadcast([128, NB, 4, 4]), op=ALU.mult))
        V(lambda: nc.vector.tensor_reduce(out=sc[:], in_=t44[:].rearrange("p b g e -> p b e g"), axis=AX.X, op=ALU.add))
        V(lambda: nc.vector.tensor_tensor(out=sc[:], in0=sc[:], in1=ein[:], op=ALU.add))
        V(lambda: nc.vector.tensor_reduce(out=m1[:], in_=sc[:], axis=AX.X, op=ALU.max))
        V(lambda: nc.vector.tensor_tensor(out=oh1[:], in0=sc[:], in1=bc3(m1[:]), op=ALU.is_equal))
        V(lambda: nc.vector.scalar_tensor_tensor(out=sc[:], in0=oh1[:], scalar=-1e30, in1=sc[:], op0=ALU.mult, op1=ALU.add))
        V(lambda: nc.vector.tensor_reduce(out=m1[:], in_=sc[:], axis=AX.X, op=ALU.max))
        V(lambda: nc.vector.tensor_tensor(out=oh2[:], in0=sc[:], in1=bc3(m1[:]), op=ALU.is_equal))
        V(lambda: nc.vector.tensor_tensor(out=sc[:], in0=oh1[:], in1=ein[:], op=ALU.mult))
        V(lambda: nc.vector.tensor_reduce(out=l1[:], in_=sc[:], axis=AX.X, op=ALU.add))
        V(lambda: nc.vector.tensor_tensor(out=sc[:], in0=oh2[:], in1=ein[:], op=ALU.mult))
        V(lambda: nc.vector.tensor_reduce(out=l2[:], in_=sc[:], axis=AX.X, op=ALU.add))
        V(lambda: nc.vector.tensor_tensor(out=l1[:], in0=l1[:], in1=l2[:], op=ALU.subtract))
        C.op(act, lambda: nc.scalar.activation(out=w1[:], in_=l1[:], func=AF.Sigmoid), reads=[t_r], writes=[t_r])
        V(lambda: nc.vector.tensor_tensor(out=w1[:], in0=w1[:], in1=psel[:], op=ALU.mult))
        V(lambda: nc.vector.tensor_tensor(out=w2[:], in0=psel[:], in1=w1[:], op=ALU.subtract))
        V(lambda: nc.vector.tensor_tensor(out=cw[:], in0=oh1[:], in1=bc3(w1[:]), op=ALU.mult))
        V(lambda: nc.vector.tensor_tensor(out=oh2[:], in0=oh2[:], in1=bc3(w2[:]), op=ALU.mult))
        V(lambda: nc.vector.tensor_tensor(out=cw[:], in0=cw[:], in1=oh2[:], op=ALU.add))
        V(lambda: nc.vector.tensor_tensor(out=comb[:], in0=ohg4, in1=cw[:].unsqueeze(2).to_broadcast([128, NB, 4, 4]), op=ALU.mult))
        for i in range(NT):
            b = i % 2
            for tb in range(4):
                blk = i * 4 + tb
                C.mm(lps[b][0:16, tb * 128:(tb + 1) * 128], [(comb[:, blk].rearrange("p g e -> p (g e)"), ident[:])],
                     reads=[t_r], writes=[t_lps[b]])
            C.op(act, lambda: nc.scalar.activation(out=cT[:, i * TT:(i + 1) * TT], in_=lps[b][0:16, :], func=AF.Copy),
                 reads=[t_lps[b]], writes=[t_cT])
        if debug:
            C.dma(sp, dbg_comb[:, :, :], comb[:].rearrange("p b g e -> p b (g e)"), reads=[t_r])
        C.barrier()
        Sr.close()

        t_hbs = [Tok() for _ in range(4)]
        t_oacc = [[Tok() for _ in range(8)] for _ in range(4)]
        for st in range(2):
            Se = Scope(C)
            hbs = Se.sb([128, 8, ST], BF16, "hbs")
            for t in range(4):
                g0 = st * ST + t * TT
                C.dma(sp, hbs[:, :, t * TT:(t + 1) * TT], hbf_d[st * 4 + t], writes=[t_hbs[t]])
            wgu = [Se.sb([128, 16, 512], BF16, "wgu") for _ in range(2)]
            wdn = [Se.sb([128, 4, D], BF16, "wdn") for _ in range(2)]
            t_wgu = [Tok(), Tok()]
            t_wdn = [Tok(), Tok()]
            cbs = [Se.sb([128, TT], F32, "cbs") for _ in range(2)]
            t_cbs = [Tok(), Tok()]
            sgb = [Se.sb([128, TT], F32, "sgb") for _ in range(2)]
            t_sgb = [Tok(), Tok()]
            t1b = [Se.sb([128, TT], F32, "t1b") for _ in range(2)]
            t_t1b = [Tok(), Tok()]
            hc = [Se.sb([128, 4, TT], BF16, "hc") for _ in range(2)]
            t_hc = [Tok(), Tok()]
            gp = [Se.ps(name="gp") for _ in range(2)]
            up = [Se.ps(name="up") for _ in range(2)]
            dp = [Se.ps(name="dp") for _ in range(2)]
            cbp = Se.ps(name="cbp")
            t_gp = [Tok(), Tok()]
            t_up = [Tok(), Tok()]
            t_dp = [Tok(), Tok()]
            t_cbp = Tok()

            def load_expert(e):
                wbuf = e % 2
                C.dma(pool, wgu[wbuf][:, 0:8, :], wg_d[l, e].rearrange("(c p) f -> p c f", p=128), writes=[t_wgu[wbuf]])
                C.dma(pool, wgu[wbuf][:, 8:16, :], wu_d[l, e].rearrange("(c p) f -> p c f", p=128), writes=[t_wgu[wbuf]])
                C.dma(pool, wdn[wbuf][:], wd_d[l, e].rearrange("(c p) d -> p c d", p=128), writes=[t_wdn[wbuf]])

            load_expert(0)

            def GU(n):
                e, t = n // 4, n % 4
                wbuf = e % 2
                if t == 1 and e + 1 < 16:
                    load_expert(e + 1)
                g0 = st * ST + t * TT
                tl = slice(t * TT, (t + 1) * TT)
                hb_i = n % 2
                C.mm(cbp[:], [(sel[:, e * 128:(e + 1) * 128], cT[:, g0:g0 + TT])], reads=[t_cT], writes=[t_cbp])
                C.op(act, lambda: nc.scalar.activation(out=cbs[hb_i][:], in_=cbp[:], func=AF.Copy), reads=[t_cbp], writes=[t_cbs[hb_i]])
                for fc in range(4):
                    k = fc % 2
                    C.mm(gp[k][:], [(wgu[wbuf][:, c, fc * 128:(fc + 1) * 128], hbs[:, c, tl]) for c in range(8)],
                         reads=[t_wgu[wbuf], t_hbs[t]], writes=[t_gp[k]])
                    C.mm(up[k][:], [(wgu[wbuf][:, 8 + c, fc * 128:(fc + 1) * 128], hbs[:, c, tl]) for c in range(8)],
                         reads=[t_wgu[wbuf], t_hbs[t]], writes=[t_up[k]])
                    C.op(act, lambda: nc.scalar.activation(out=sgb[k][:], in_=gp[k][:], func=AF.Silu), reads=[t_gp[k]], writes=[t_sgb[k]])
                    C.op(dve, lambda: nc.vector.tensor_tensor(out=t1b[k][:], in0=up[k][:], in1=sgb[k][:], op=ALU.mult),
                         reads=[t_up[k], t_sgb[k]], writes=[t_t1b[k]])
                    C.op(pool, lambda: nc.gpsimd.tensor_tensor(out=hc[hb_i][:, fc, :], in0=t1b[k][:], in1=cbs[hb_i][:], op=ALU.mult),
                         reads=[t_t1b[k], t_cbs[hb_i]], writes=[t_hc[hb_i]])

            def DN(n):
                e, t = n // 4, n % 4
                wbuf = e % 2
                tl = slice(t * TT, (t + 1) * TT)
                hb_i = n % 2
                for dc in range(8):
                    k = dc % 2
                    C.mm(dp[k][:], [(wdn[wbuf][:, fc, dc * 128:(dc + 1) * 128], hc[hb_i][:, fc, :]) for fc in range(4)],
                         reads=[t_wdn[wbuf], t_hc[hb_i]], writes=[t_dp[k]])
                    if e == 0:
                        C.op(act, lambda: nc.scalar.activation(out=oacc[:, dc, tl], in_=dp[k][:], func=AF.Copy),
                             reads=[t_dp[k]], writes=[t_oacc[t][dc]])
                    else:
                        C.op(dve, lambda: nc.vector.tensor_tensor(out=oacc[:, dc, tl], in0=oacc[:, dc, tl], in1=dp[k][:], op=ALU.add),
                             reads=[t_dp[k], t_oacc[t][dc]], writes=[t_oacc[t][dc]])
            pipeline(64, [GU, DN])
            C.barrier()
            Se.close()
            Sl = Scope(C)
            L = ln_alloc(Sl, 2, final)
            xs = [Sl.sb([128, 8, TT], F32, "xs3") for _ in range(3)]
            t_xs = [Tok() for _ in range(3)]

            def T0(t):
                C.dma(sp, xs[t % 3][:], h32_d[st * 4 + t], writes=[t_xs[t % 3]])

            def T1(t):
                x3 = t % 3
                tl = slice(t * TT, (t + 1) * TT)
                C.op(dve, lambda: nc.vector.scalar_tensor_tensor(out=xs[x3][:], in0=xs[x3][:], scalar=ALPHA, in1=oacc[:, :, tl],
                                                                 op0=ALU.mult, op1=ALU.add), reads=[t_xs[x3]] + t_oacc[t], writes=[t_xs[x3]])
                ln_s1(L, t, xs[x3], t_xs[x3])

            def T2(t):
                ln_s2(L, t)

            def T3(t):
                ln_s3(L, t, xs[t % 3], t_xs[t % 3], vcol(l, 16, 8), vcol(l, 24, 8), st * 4 + t, final)
            pipeline(4, [T0, T1, T2, T3], reverse=True)
            C.barrier()
            Sl.close()
        Sm.close()

    dbg_comb = None
    if debug:
        dbg_comb = nc.dram_tensor("dbg_comb", [128, 32, 16], F32, kind="ExternalOutput").ap()

    def run():
        phase0()
        if stop == "p0":
            return
        for l in range(nlayers):
            Sh = Scope(C)
            hb, hb_t = load_hbf(Sh)
            phaseA(l, hb, hb_t)
            if stop == "A":
                Sh.close()
                return
            phaseB(l, hb, hb_t)
            if stop == "B":
                Sh.close()
                return
            phaseC(l, hb, hb_t)
            if stop == "C":
                Sh.close()
                return
            phaseD1(l, hb, hb_t)
            Sh.close()
            if stop == "D1":
                return
            phaseD2(l)
            if stop == "D2":
                return
            phaseE(l, final=(l == nlayers - 1 and stop is None))
            if stop == "E":
                return

    run()
    C.barrier()
    G.close()
    C.es.close()
    return nc


def _fmaj(v, n):
    return np.ascontiguousarray(np.asarray(v, np.float32).reshape(n, 128).T)


def prepare_inputs(inputs):
    f32 = np.float32
    g = {k: np.asarray(v) for k, v in inputs.items()}
    vecs = np.zeros((DEPTH, 128, NV), f32)
    rb = np.zeros((DEPTH, 128, 20), f32)
    wa_bd = np.zeros((DEPTH, 4, 128, 128), f32)
    wx_bd = np.zeros((DEPTH, 4, 128, 128), f32)
    for l in range(DEPTH):
        vecs[l, :, 0:8] = _fmaj(g["ln1_g"][l], 8)
        vecs[l, :, 8:16] = _fmaj(g["ln1_b"][l], 8)
        vecs[l, :, 16:24] = _fmaj(g["ln2_g"][l], 8)
        vecs[l, :, 24:32] = _fmaj(g["ln2_b"][l], 8)
        for br in range(3):
            vecs[l, :, 32 + br * 8:40 + br * 8] = _fmaj(g["gate_b"][l, br], 8)
        for k in range(4):
            vecs[l, :, 56 + k:72:4] = _fmaj(g["lru_conv_w"][l, k], 4)
        vecs[l, :, 72:76] = _fmaj(g["lru_conv_b"][l], 4)
        vecs[l, :, 76:80] = _fmaj(g["lru_ba"][l], 4)
        vecs[l, :, 80:84] = _fmaj(g["lru_bx"][l], 4)
        vecs[l, :, 84:88] = _fmaj(g["lru_lambda"][l], 4)
        for k in range(3):
            vecs[l, :, 88 + k:100:3] = _fmaj(g["sc_conv_w"][l, k], 4)
        rb[l, :, 0:4] = g["group_bias"][l][None, :]
        rb[l, :, 4:20] = g["expert_bias"][l][None, :]
        for c in range(4):
            for h in range(2):
                wa_bd[l, c, h * 64:(h + 1) * 64, h * 64:(h + 1) * 64] = g["lru_wa"][l, 2 * c + h]
                wx_bd[l, c, h * 64:(h + 1) * 64, h * 64:(h + 1) * 64] = g["lru_wx"][l, 2 * c + h]
    lnin = np.concatenate([_fmaj(g["ln_in_g"], 8), _fmaj(g["ln_in_b"], 8)], axis=1)
    w_branch = np.ascontiguousarray(np.stack([g["w_branch_sb"], g["w_branch_lru"], g["w_branch_sc"]], axis=1).astype(f32))
    w_router = np.ascontiguousarray(np.concatenate([g["w_group"], g["w_expert_router"]], axis=2).astype(f32))
    ident = np.eye(128, dtype=f32)
    jj, ss = np.meshgrid(np.arange(128), np.arange(128), indexing="ij")
    negtri = np.where(jj >= ss, -1.0, 0.0).astype(f32)
    sidx = np.arange(128)[:, None]
    tidx = np.arange(512)[None, :]
    masks = np.concatenate([(tidx > sidx + 128 * j).astype(f32) for j in (0, 0, 1, 1, 2, 2, 3, 3)], axis=1)
    sel = np.zeros((16, 16, 128), f32)
    for e in range(16):
        sel[e, e, :] = 1.0
    sel = sel.reshape(16, 16 * 128)
    shared = dict(w_in=np.ascontiguousarray(g["w_in"].astype(f32)), wa_bd=wa_bd, wx_bd=wx_bd, w_branch=w_branch,
                  w_out=np.ascontiguousarray(g["w_out"].astype(f32)), w_router=w_router,
                  w_gate=np.ascontiguousarray(g["w_gate"].astype(f32)), w_up=np.ascontiguousarray(g["w_up"].astype(f32)),
                  w_down=np.ascontiguousarray(g["w_down"].astype(f32)), vecs=vecs, lnin=np.ascontiguousarray(lnin),
                  rbias=rb, ident=ident, negtri=negtri, masks=np.ascontiguousarray(masks), sel=sel)
    x = np.asarray(g["x"], f32)
    in_maps = []
    for c in range(NCORES):
        m = dict(shared)
        m["x"] = np.ascontiguousarray(x[c])
        in_maps.append(m)
    return in_maps


def kernel(**inputs):
    in_maps = prepare_inputs(inputs)
    nc = build()
    res = run_bass_kernel_spmd(nc, in_maps, core_ids=list(range(NCORES)))
    return np.stack([np.asarray(res.results[c]["out"], np.float32) for c in range(NCORES)], axis=0)
```

```python
import numpy as np
from contextlib import ExitStack
import concourse.bass as bass
import concourse.mybir as mybir
from concourse.bass_utils import run_bass_kernel_spmd

F32 = mybir.dt.float32
BF16 = mybir.dt.bfloat16
AF = mybir.ActivationFunctionType
ALU = mybir.AluOpType
AX = mybir.AxisListType

S = 4096
D = 1024
PC = 7168
NT = 8
TT = 512
DEPTH = 2
ALPHA = float((2 * DEPTH) ** 0.25)
LN_EPS = 1e-5
NV = 100
NCORES = 8


class Tok:
    __slots__ = ("w", "r")

    def __init__(self):
        self.w = None
        self.r = {}


class Eng:
    def __init__(self, ctx, eng, name, is_pe=False):
        self.ctx = ctx
        self.eng = eng
        self.name = name
        self.is_pe = is_pe
        self.sem = ctx.es.enter_context(ctx.nc.semaphore("s_" + name))
        self.count = 0
        self.waited = {}

    def wait(self, ev):
        sem, val, src = ev
        if src is self and self.is_pe:
            return
        if self.waited.get(sem, 0) >= val:
            return
        self.eng.wait_ge(sem, val)
        self.waited[sem] = val

    def signal(self, ins):
        self.count += 1
        ins.then_inc(self.sem, 1)
        return (self.sem, self.count, self)


class Ctx:
    NDMA = 24

    def __init__(self, nc):
        self.nc = nc
        self.es = ExitStack()
        self.pe = Eng(self, nc.tensor, "pe", True)
        self.act = Eng(self, nc.scalar, "act")
        self.dve = Eng(self, nc.vector, "dve")
        self.pool = Eng(self, nc.gpsimd, "pool")
        self.sp = Eng(self, nc.sync, "sp")
        self.engs = [self.pe, self.act, self.dve, self.pool, self.sp]
        self.dsem = [[self.es.enter_context(nc.semaphore("d%d" % i)), 0] for i in range(self.NDMA)]
        self.di = {}
        self.uid = 0

    def _deps(self, E, reads, writes):
        for b in reads:
            if b.w is not None:
                E.wait(b.w)
        for b in writes:
            if b.w is not None:
                E.wait(b.w)
            for ev in b.r.values():
                E.wait(ev)

    def _reg(self, ev, reads, writes):
        for b in reads:
            b.r[ev[0]] = ev
        for b in writes:
            b.w = ev
            b.r = {}

    def op(self, E, fn, reads=(), writes=()):
        self._deps(E, reads, writes)
        ins = fn()
        ev = E.signal(ins)
        self._reg(ev, reads, writes)
        return ev

    def dma(self, Q, out_ap, in_ap, reads=(), writes=()):
        self._deps(Q, reads, writes)
        half = self.NDMA // 2
        base = 0 if Q is self.sp else half
        n = self.di.get(base, 0)
        self.di[base] = n + 1
        slot = self.dsem[base + n % half]
        if slot[1] > 0:
            Q.wait((slot[0], 16 * slot[1], None))
        ins = Q.eng.dma_start(out=out_ap, in_=in_ap)
        slot[1] += 1
        ins.then_inc(slot[0], 16)
        ev = (slot[0], 16 * slot[1], None)
        self._reg(ev, reads, writes)
        return ev

    def mm(self, out_ap, pairs, reads=(), writes=(), start=True, stop=True):
        n = len(pairs)

        def fn():
            ins = None
            for i, (l, r) in enumerate(pairs):
                ins = self.nc.tensor.matmul(out_ap, l, r, start=(start and i == 0), stop=(stop and i == n - 1))
            return ins
        return self.op(self.pe, fn, reads, writes)

    def barrier(self):
        evs = [(E.sem, E.count, E) for E in self.engs if E.count > 0]
        evs += [(s[0], 16 * s[1], None) for s in self.dsem if s[1] > 0]
        for E in self.engs:
            for ev in evs:
                if ev[2] is not E:
                    E.wait(ev)

    def name(self, p):
        self.uid += 1
        return "%s_%d" % (p, self.uid)


class Scope:
    def __init__(self, ctx):
        self.ctx = ctx
        self.es = ExitStack()

    def sb(self, shape, dt, name="t"):
        return self.es.enter_context(self.ctx.nc.sbuf_tensor(self.ctx.name(name), list(shape), dt))

    def ps(self, shape=(128, 512), dt=F32, name="p"):
        return self.es.enter_context(self.ctx.nc.psum_tensor(self.ctx.name(name), list(shape), dt))

    def close(self):
        self.es.close()


def build(nlayers=DEPTH, stop=None, debug=False):
    nc = bass.Bass("TRN2", target_bir_lowering=False)

    def din(name, shape, dt=F32):
        return nc.dram_tensor(name, list(shape), dt, kind="ExternalInput").ap()

    def dscr(name, shape, dt):
        return nc.dram_tensor(name, list(shape), dt, kind=("ExternalOutput" if debug else "Internal")).ap()

    x_d = din("x", [S, D])
    w_in_d = din("w_in", [DEPTH, D, PC])
    wa_d = din("wa_bd", [DEPTH, 4, 128, 128])
    wx_d = din("wx_bd", [DEPTH, 4, 128, 128])
    wbr_d = din("w_branch", [DEPTH, 3, 512, D])
    wout_d = din("w_out", [DEPTH, D, D])
    wr_d = din("w_router", [DEPTH, D, 20])
    wg_d = din("w_gate", [DEPTH, 16, D, 512])
    wu_d = din("w_up", [DEPTH, 16, D, 512])
    wd_d = din("w_down", [DEPTH, 16, 512, D])
    vecs_d = din("vecs", [DEPTH, 128, NV])
    lnin_d = din("lnin", [128, 16])
    rb_d = din("rbias", [DEPTH, 128, 20])
    ident_d = din("ident", [128, 128])
    negtri_d = din("negtri", [128, 128])
    masks_d = din("masks", [128, 8 * 512])
    sel_d = din("sel", [16, 16 * 128])
    out_d = nc.dram_tensor("out", [S, D], F32, kind="ExternalOutput").ap()

    h32_d = dscr("h32", [NT, 128, 8, TT], F32)
    hbf_d = dscr("hbf", [NT, 128, 8, TT], BF16)
    ysb_d = dscr("ysb", [512, S], BF16)
    ylru_d = dscr("ylru", [512, S], BF16)
    ysc_d = dscr("ysc", [512, S], BF16)
    mT_d = dscr("mT", [NT, 128, 8, TT], BF16)

    def fm(ap):
        return ap.rearrange("(c p) t -> p c t", p=128)

    C = Ctx(nc)
    pe, act, dve, pool, sp = C.engs

    G = Scope(C)
    ident = G.sb([128, 128], F32, "ident")
    negtri = G.sb([128, 128], BF16, "negtri")
    ones_bf = G.sb([128, 128], BF16, "ones")
    negones = G.sb([128, 128], BF16, "negones")
    mean_bf = G.sb([128, 128], BF16, "meanm")
    masks = G.sb([128, 8 * 512], BF16, "masks")
    sel = G.sb([16, 16 * 128], BF16, "sel")
    lnin = G.sb([128, 16], F32, "lnin")
    vecs = G.sb([128, DEPTH * NV], F32, "vecs")
    rbias = G.sb([128, DEPTH * 20], F32, "rbias")
    cchan = G.sb([128, DEPTH * 4], F32, "cchan")
    wr_sb = G.sb([128, 8, 20], F32, "wr")
    lneps = G.sb([128, 1], F32, "lneps")
    lg = G.sb([128, 32, 20], F32, "lg")
    t_wr, t_lg = Tok(), Tok()
    k_const = Tok()
    C.dma(sp, ident[:], ident_d[:, :], writes=[k_const])
    C.dma(pool, negtri[:], negtri_d[:, :], writes=[k_const])
    C.dma(pool, masks[:], masks_d[:, :], writes=[k_const])
    C.dma(pool, sel[:], sel_d[:, :], writes=[k_const])
    C.dma(sp, lnin[:], lnin_d[:, :], writes=[k_const])
    for l in range(DEPTH):
        C.dma(sp, vecs[:, l * NV:(l + 1) * NV], vecs_d[l], writes=[k_const])
        C.dma(sp, rbias[:, l * 20:(l + 1) * 20], rb_d[l], writes=[k_const])
    C.op(dve, lambda: nc.vector.memset(ones_bf[:], 1.0), writes=[k_const])
    C.op(dve, lambda: nc.vector.memset(mean_bf[:], 1.0 / D), writes=[k_const])
    C.op(dve, lambda: nc.vector.memset(negones[:], -1.0), writes=[k_const])
    C.op(dve, lambda: nc.vector.memset(lneps[:], LN_EPS), writes=[k_const])
    C.barrier()
    for l in range(DEPTH):
        C.op(act, lambda l=l: nc.scalar.activation(out=cchan[:, l * 4:(l + 1) * 4], in_=vecs[:, l * NV + 84:l * NV + 88],
                                                   func=AF.Exp, scale=-1.0), reads=[k_const], writes=[k_const])
        C.op(act, lambda l=l: nc.scalar.activation(out=cchan[:, l * 4:(l + 1) * 4], in_=cchan[:, l * 4:(l + 1) * 4],
                                                   func=AF.Ln, bias=1.0), reads=[k_const], writes=[k_const])
    C.op(dve, lambda: nc.vector.tensor_scalar(out=cchan[:], in0=cchan[:], scalar1=-8.0, scalar2=None, op0=ALU.mult),
         reads=[k_const], writes=[k_const])
    C.barrier()

    def vcol(l, c0, n=1):
        return vecs[:, l * NV + c0:l * NV + c0 + n]

    def ln_alloc(Sc, nsets, final=False):
        L = {"sets": [], "ps": [], "n": nsets}
        for _ in range(nsets):
            W = {}
            W["xb"] = Sc.sb([128, 8, TT], BF16, "lnxb")
            W["sq"] = Sc.sb([128, 8, TT], BF16, "lnsq")
            W["hb"] = Sc.sb([128, 8, TT], BF16, "lnhb")
            W["mean_sb"] = Sc.sb([128, TT], F32, "lnmean")
            W["msq"] = Sc.sb([128, TT], F32, "lnmsq")
            W["rstd"] = Sc.sb([128, TT], F32, "lnrstd")
            for k in ("t_xb", "t_sq", "t_hb", "t_st"):
                W[k] = Tok()
            W["t_c"] = [Tok() for _ in range(8)]
            L["sets"].append(W)
        for _ in range(2):
            L["ps"].append(dict(mean_ps=Sc.ps(name="lnmps"), ex2_ps=Sc.ps(name="lneps"), t_mps=Tok(), t_eps=Tok()))
        if final:
            L["ot"] = [Sc.sb([128, D], F32, "lnot") for _ in range(2)]
            L["t_ot"] = [Tok(), Tok()]
            L["tp_ps"] = [Sc.ps(name="lntp") for _ in range(2)]
            L["t_tp"] = [Tok(), Tok()]
        return L

    def ln_s1(L, k, xs, xs_tok):
        W = L["sets"][k % L["n"]]
        Pp = L["ps"][k % 2]
        xb, sq = W["xb"], W["sq"]
        C.op(dve, lambda: nc.vector.tensor_copy(out=xb[:], in_=xs[:]), reads=[xs_tok], writes=[W["t_xb"]])
        C.op(act, lambda: nc.scalar.activation(out=sq[:], in_=xs[:], func=AF.Square), reads=[xs_tok], writes=[W["t_sq"]])
        C.mm(Pp["mean_ps"][:], [(mean_bf[:], xb[:, c, :]) for c in range(8)], reads=[W["t_xb"]], writes=[Pp["t_mps"]])
        C.mm(Pp["ex2_ps"][:], [(mean_bf[:], sq[:, c, :]) for c in range(8)], reads=[W["t_sq"]], writes=[Pp["t_eps"]])

    def ln_s2(L, k):
        W = L["sets"][k % L["n"]]
        Pp = L["ps"][k % 2]
        mean_sb, msq, rstd, t_st = W["mean_sb"], W["msq"], W["rstd"], W["t_st"]
        C.op(act, lambda: nc.scalar.activation(out=mean_sb[:], in_=Pp["mean_ps"][:], func=AF.Copy), reads=[Pp["t_mps"]], writes=[t_st])
        C.op(act, lambda: nc.scalar.activation(out=msq[:], in_=Pp["mean_ps"][:], func=AF.Square), reads=[Pp["t_mps"]], writes=[t_st])
        C.op(dve, lambda: nc.vector.tensor_tensor(out=rstd[:], in0=Pp["ex2_ps"][:], in1=msq[:], op=ALU.subtract),
             reads=[Pp["t_eps"], t_st], writes=[t_st])
        C.op(act, lambda: nc.scalar.activation(out=rstd[:], in_=rstd[:], func=AF.Ln, bias=lneps[:, 0:1]), reads=[t_st], writes=[t_st])
        C.op(act, lambda: nc.scalar.activation(out=rstd[:], in_=rstd[:], func=AF.Exp, scale=-0.5), reads=[t_st], writes=[t_st])

    def ln_s3(L, k, xs, xs_tok, gcol, bcol, tile_i, final, router=None):
        W = L["sets"][k % L["n"]]
        hb, t_hb, t_st = W["hb"], W["t_hb"], W["t_st"]
        mean_sb, rstd = W["mean_sb"], W["rstd"]
        for c in range(8):
            E = pool if c in (3, 7) else dve
            ee = E.eng
            tk = W["t_c"][c]
            C.op(E, lambda: ee.tensor_tensor(out=xs[:, c, :], in0=xs[:, c, :], in1=mean_sb[:], op=ALU.subtract),
                 reads=[t_st, xs_tok, W["t_xb"], W["t_sq"]], writes=[tk])
            C.op(E, lambda: ee.tensor_tensor(out=xs[:, c, :], in0=xs[:, c, :], in1=rstd[:], op=ALU.mult),
                 reads=[t_st, tk], writes=[tk])
            if not final:
                C.op(act, lambda: nc.scalar.activation(out=hb[:, c, :], in_=xs[:, c, :], func=AF.Identity,
                                                       scale=gcol[:, c:c + 1], bias=bcol[:, c:c + 1]), reads=[tk], writes=[t_hb])
            C.op(act, lambda: nc.scalar.activation(out=xs[:, c, :], in_=xs[:, c, :], func=AF.Identity,
                                                   scale=gcol[:, c:c + 1], bias=bcol[:, c:c + 1]), reads=[tk], writes=[tk])
        tsl = slice(tile_i * TT, (tile_i + 1) * TT)
        if router is not None:
            wr, t_wr, lpsL, t_lpsL, lg, t_lg, lg_copy = router
            if tile_i > 0:
                lg_copy(tile_i - 1)
            lps, t_lps = lpsL[tile_i % 2], t_lpsL[tile_i % 2]
            for tb in range(4):
                C.mm(lps[:, tb * 32:tb * 32 + 20], [(xs[:, c, tb * 128:(tb + 1) * 128], wr[:, c, :]) for c in range(8)],
                     reads=W["t_c"] + [t_wr], writes=[t_lps])
        if not final:
            C.dma(sp, h32_d[tile_i], xs[:], writes=[xs_tok] + W["t_c"])
            C.dma(sp, hbf_d[tile_i], hb[:], reads=[t_hb])
        else:
            ot, t_ot, tp_ps, t_tp = L["ot"], L["t_ot"], L["tp_ps"], L["t_tp"]
            for tb in range(4):
                for half in range(2):
                    kk = (tb * 2 + half) % 2
                    for cc in range(4):
                        c = half * 4 + cc
                        C.mm(tp_ps[kk][:, cc * 128:(cc + 1) * 128], [(xs[:, c, tb * 128:(tb + 1) * 128], ident[:])],
                             reads=[W["t_c"][c], xs_tok], writes=[t_tp[kk]])
                    C.op(act, lambda: nc.scalar.activation(out=ot[tb % 2][:, half * 512:(half + 1) * 512], in_=tp_ps[kk][:], func=AF.Copy),
                         reads=[t_tp[kk]], writes=[t_ot[tb % 2]])
                r0 = tile_i * TT + tb * 128
                C.dma(sp, out_d[r0:r0 + 128, :], ot[tb % 2][:], reads=[t_ot[tb % 2]])

    def pipeline(n, stages, reverse=False):
        ns = len(stages)
        order = list(enumerate(stages))
        if reverse:
            order = order[::-1]
        for step in range(n + ns - 1):
            for si, st in order:
                if 0 <= step - si < n:
                    st(step - si)

    def phase0():
        Sc = Scope(C)
        L = ln_alloc(Sc, 2)
        xt = [Sc.sb([128, 4, D], F32, "xt") for _ in range(2)]
        t_xt = [Tok(), Tok()]
        xs = [Sc.sb([128, 8, TT], F32, "xs") for _ in range(3)]
        t_xs = [Tok() for _ in range(3)]
        tp = [Sc.ps(name="tp") for _ in range(2)]
        t_tp = [Tok(), Tok()]
        xv = x_d.rearrange("(n tb p) d -> n p tb d", tb=4, p=128)

        def T0(i):
            C.dma(sp, xt[i % 2][:], xv[i], writes=[t_xt[i % 2]])

        def T1(i):
            b = i % 2
            x3 = i % 3
            for c in range(8):
                k = c % 2
                for tb in range(4):
                    C.mm(tp[k][:, tb * 128:(tb + 1) * 128], [(xt[b][:, tb, c * 128:(c + 1) * 128], ident[:])],
                         reads=[t_xt[b]], writes=[t_tp[k]])
                if c % 2 == 0:
                    C.op(act, lambda: nc.scalar.activation(out=xs[x3][:, c, :], in_=tp[k][:], func=AF.Copy),
                         reads=[t_tp[k]], writes=[t_xs[x3]])
                else:
                    C.op(dve, lambda: nc.vector.tensor_copy(out=xs[x3][:, c, :], in_=tp[k][:]), reads=[t_tp[k]], writes=[t_xs[x3]])
            ln_s1(L, i, xs[x3], t_xs[x3])

        def T2(i):
            ln_s2(L, i)

        def T3(i):
            ln_s3(L, i, xs[i % 3], t_xs[i % 3], lnin[:, 0:8], lnin[:, 8:16], i, False)
        pipeline(NT, [T0, T1, T2, T3], reverse=True)
        C.barrier()
        Sc.close()

    def load_w_cols(dst, tok, l, col0, ncols):
        src = w_in_d[l].rearrange("(c p) e -> p c e", p=128)[:, :, col0:col0 + ncols]
        C.dma(pool, dst[:], src, writes=[tok])

    def load_hbf(Sc):
        hb = Sc.sb([128, 8, S], BF16, "hbres")
        toks = [Tok() for _ in range(NT)]
        for i in range(NT):
            C.dma(sp, hb[:, :, i * TT:(i + 1) * TT], hbf_d[i], writes=[toks[i]])
        return hb, toks

    def phaseA(l, hb, hb_t):
        Sc = Scope(C)
        wq = [Sc.sb([128, 8, 384], BF16, "wqkv") for _ in range(2)]
        t_wq = [Tok(), Tok()]
        qT2 = [Sc.sb([128, S], BF16, "qT") for _ in range(2)]
        kT2 = [Sc.sb([128, S], BF16, "kT") for _ in range(2)]
        vv2 = [Sc.sb([128, 32, 128], BF16, "vv") for _ in range(2)]
        t_q2 = [[Tok() for _ in range(NT)] for _ in range(2)]
        t_k2 = [[Tok() for _ in range(NT)] for _ in range(2)]
        t_v2 = [[Tok() for _ in range(NT)] for _ in range(2)]
        W2 = 2 * TT
        NU, NSP, NE, NW = 2, 5, 3, 4
        u_sb = [Sc.sb([128, W2], F32, "u") for _ in range(NU)]
        sp_sb = [Sc.sb([128, W2], BF16, "spl") for _ in range(NSP)]
        e_sb = [Sc.sb([128, W2], F32, "e") for _ in range(NE)]
        w_sb = [Sc.sb([128, W2], BF16, "w") for _ in range(NW)]
        carry = [Sc.sb([128, W2], F32, "carry") for _ in range(2)]
        yo = [Sc.sb([128, TT], BF16, "yo") for _ in range(2)]
        t_u = [Tok() for _ in range(NU)]
        t_sp = [Tok() for _ in range(NSP)]
        t_e = [Tok() for _ in range(NE)]
        t_w = [Tok() for _ in range(NW)]
        t_carry = [Tok(), Tok()]
        t_yo = [Tok(), Tok()]
        _z = Sc.ps([128, W2], name="z")
        z_ps = [_z, _z]
        a_ps = Sc.ps([128, W2], name="a")
        c_ps = Sc.ps([128, W2], name="c")
        pv_ps = Sc.ps(name="pv")
        pj_ps = Sc.ps(name="pj")
        t_pj = Tok()
        _tz = Tok()
        t_z = [_tz, _tz]
        t_a = Tok()
        t_c = Tok()
        t_pv = Tok()

        def load_wq(p):
            for j in range(3):
                src = w_in_d[l].rearrange("(c p) e -> p c e", p=128)[:, :, j * 512 + p * 128: j * 512 + (p + 1) * 128]
                C.dma(pool, wq[p % 2][:, :, j * 128:(j + 1) * 128], src, writes=[t_wq[p % 2]])

        def proj_units(p):
            wb = p % 2
            qd, kd, vd = qT2[wb], kT2[wb], vv2[wb]
            units = []
            for i in range(NT):
                tsl = slice(i * TT, (i + 1) * TT)

                def uq(i=i, tsl=tsl):
                    C.mm(pj_ps[:], [(wq[wb][:, c, 0:128], hb[:, c, tsl]) for c in range(8)],
                         reads=[t_wq[wb], hb_t[i]], writes=[t_pj])
                    C.op(dve, lambda: nc.vector.tensor_scalar(out=qd[:, tsl], in0=pj_ps[:], scalar1=0.125, scalar2=None, op0=ALU.mult),
                         reads=[t_pj], writes=[t_q2[wb][i]])

                def uk(i=i, tsl=tsl):
                    C.mm(pj_ps[:], [(wq[wb][:, c, 128:256], hb[:, c, tsl]) for c in range(8)],
                         reads=[t_wq[wb], hb_t[i]], writes=[t_pj])
                    C.op(dve, lambda: nc.vector.tensor_copy(out=kd[:, tsl], in_=pj_ps[:]), reads=[t_pj], writes=[t_k2[wb][i]])

                def uv(i=i):
                    for tb in range(4):
                        blk = i * 4 + tb
                        C.mm(pj_ps[:, tb * 128:(tb + 1) * 128],
                             [(hb[:, c, blk * 128:(blk + 1) * 128], wq[wb][:, c, 256:384]) for c in range(8)],
                             reads=[t_wq[wb], hb_t[i]], writes=[t_pj])
                    C.op(dve, lambda: nc.vector.tensor_copy(out=vd[:, i * 4:(i + 1) * 4, :],
                                                            in_=pj_ps[:].rearrange("p (a b) -> p a b", b=128)),
                         reads=[t_pj], writes=[t_v2[wb][i]])
                units += [uq, uk, uv]
            return units

        load_wq(0)
        units0 = proj_units(0)
        for p in range(4):
            wb = p % 2
            qT, kT, vv = qT2[wb], kT2[wb], vv2[wb]
            t_q, t_k, t_v = t_q2[wb], t_k2[wb], t_v2[wb]
            nxt = []
            if p + 1 < 4:
                load_wq(p + 1)
                nxt = proj_units(p + 1)
            steps = []
            sweep = 0
            for i in range(NT):
                nk = 4 * (i + 1)
                for m in range(nk):
                    kb = nk - 1 - m
                    steps.append(dict(i=i, kb=kb, first=(m == 0), last=(m == nk - 1), dj=(kb - 4 * i if kb >= 4 * i else None), sw=sweep))
                sweep += 1
            NST = len(steps)
            h0, h1 = slice(0, 64), slice(64, 128)

            def S1(n):
                T = steps[n]
                tsl = slice(T["i"] * TT, (T["i"] + 1) * TT)
                ks = slice(T["kb"] * 128, (T["kb"] + 1) * 128)
                ru, r = n % NU, n % NSP

                def fz():
                    nc.tensor.matmul(z_ps[0][:, 0:TT], kT[h0, ks], qT[h0, tsl], start=True, stop=True)
                    return nc.tensor.matmul(z_ps[0][:, TT:W2], kT[h1, ks], qT[h1, tsl], start=True, stop=True)
                C.op(pe, fz, reads=[t_k[T["kb"] // 4], t_q[T["i"]]], writes=[t_z[0]])
                C.op(act, lambda: nc.scalar.activation(out=u_sb[ru][:], in_=z_ps[0][:], func=AF.Exp), reads=[t_z[0]], writes=[t_u[ru]])
                C.op(act, lambda: nc.scalar.activation(out=sp_sb[r][:], in_=u_sb[ru][:], func=AF.Ln, bias=1.0),
                     reads=[t_u[ru]], writes=[t_sp[r]])
                if T["dj"] is not None:
                    m = masks[:, T["dj"] * W2:(T["dj"] + 1) * W2]
                    C.op(dve, lambda: nc.vector.tensor_tensor(out=sp_sb[r][:], in0=sp_sb[r][:], in1=m, op=ALU.mult),
                         reads=[t_sp[r]], writes=[t_sp[r]])

            def S2(n):
                T = steps[n]
                tsl = slice(T["i"] * TT, (T["i"] + 1) * TT)
                ks = slice(T["kb"] * 128, (T["kb"] + 1) * 128)
                r, re, rw = n % NSP, n % NE, n % NW
                cy = carry[T["sw"] % 2]
                t_cy = t_carry[T["sw"] % 2]
                sp1 = sp_sb[r][:, 0:TT]
                sp2 = sp_sb[r][:, TT:W2]

                def fa():
                    nc.tensor.matmul(a_ps[:, 0:TT], kT[h0, ks], qT[h0, tsl], start=True, stop=False)
                    nc.tensor.matmul(a_ps[:, TT:W2], kT[h1, ks], qT[h1, tsl], start=True, stop=False)
                    nc.tensor.matmul(a_ps[:, 0:TT], negtri[:], sp1, start=False, stop=True)
                    return nc.tensor.matmul(a_ps[:, TT:W2], negtri[:], sp2, start=False, stop=True)
                C.op(pe, fa, reads=[t_sp[r], t_k[T["kb"] // 4], t_q[T["i"]]], writes=[t_a])
                if not T["last"]:
                    def fc():
                        nc.tensor.matmul(c_ps[:, 0:TT], ones_bf[:], sp1, start=True, stop=True)
                        return nc.tensor.matmul(c_ps[:, TT:W2], ones_bf[:], sp2, start=True, stop=True)
                    C.op(pe, fc, reads=[t_sp[r]], writes=[t_c])
                if T["first"]:
                    C.op(dve, lambda: nc.vector.tensor_copy(out=e_sb[re][:], in_=a_ps[:]), reads=[t_a], writes=[t_e[re]])
                    if not T["last"]:
                        C.op(dve, lambda: nc.vector.tensor_scalar(out=cy[:], in0=c_ps[:], scalar1=-1.0, scalar2=None, op0=ALU.mult),
                             reads=[t_c], writes=[t_cy])
                else:
                    C.op(dve, lambda: nc.vector.tensor_tensor(out=e_sb[re][:], in0=a_ps[:], in1=cy[:], op=ALU.add),
                         reads=[t_a, t_cy], writes=[t_e[re]])
                    if not T["last"]:
                        C.op(dve, lambda: nc.vector.tensor_tensor(out=cy[:], in0=cy[:], in1=c_ps[:], op=ALU.subtract),
                             reads=[t_c, t_cy], writes=[t_cy])
                C.op(act, lambda: nc.scalar.activation(out=w_sb[rw][:], in_=e_sb[re][:], func=AF.Exp), reads=[t_e[re]], writes=[t_w[rw]])
                if T["dj"] is not None:
                    m = masks[:, T["dj"] * W2:(T["dj"] + 1) * W2]
                    C.op(pool, lambda: nc.gpsimd.tensor_tensor(out=w_sb[rw][:], in0=w_sb[rw][:], in1=m, op=ALU.mult),
                         reads=[t_w[rw]], writes=[t_w[rw]])

            def S3(n):
                T = steps[n]
                rw = n % NW
                pb = T["sw"] % 2

                def fp():
                    nc.tensor.matmul(pv_ps[0:64, :], vv[:, T["kb"], 0:64], w_sb[rw][:, 0:TT], start=T["first"], stop=T["last"])
                    return nc.tensor.matmul(pv_ps[64:128, :], vv[:, T["kb"], 64:128], w_sb[rw][:, TT:W2], start=T["first"], stop=T["last"])
                C.op(pe, fp, reads=[t_w[rw], t_v[T["kb"] // 4], t_pv], writes=[t_pv])
                if T["last"]:
                    C.op(dve, lambda: nc.vector.tensor_copy(out=yo[pb][:], in_=pv_ps[:]), reads=[t_pv], writes=[t_yo[pb]])
                    C.dma(sp, ysb_d[p * 128:(p + 1) * 128, T["i"] * TT:(T["i"] + 1) * TT], yo[pb][:], reads=[t_yo[pb]])

            for n in range(NST + 5):
                if n < NST:
                    if p == 0 and steps[n]["first"]:
                        for _ in range(3):
                            units0.pop(0)()
                    S1(n)
                if 0 <= n - 3 < NST:
                    S2(n - 3)
                if 0 <= n - 5 < NST:
                    S3(n - 5)
                if nxt and n % 5 == 4:
                    nxt.pop(0)()
            while nxt:
                nxt.pop(0)()
        C.barrier()
        Sc.close()

    def phaseB(l, hb, hb_t):
        Sc = Scope(C)
        wl = [Sc.sb([128, 8, 256], BF16, "wl") for _ in range(2)]
        t_wl = [Tok(), Tok()]
        wab = [Sc.sb([128, 256], BF16, "wab") for _ in range(2)]
        t_wab = [Tok(), Tok()]
        lin = Sc.sb([128, 3 + S], F32, "lin")
        ub = Sc.sb([128, S], F32, "ub")
        rb_ = Sc.sb([128, S], F32, "rb")
        ib = Sc.sb([128, S], F32, "ib")
        ubf = Sc.sb([128, S], BF16, "ubf")
        yb = Sc.sb([128, S], BF16, "yb")
        NH = 2
        HS = S // NH
        t_lin = Tok()
        t_ub = [Tok() for _ in range(NH)]
        t_rb = [Tok() for _ in range(NH)]
        t_ib = [Tok() for _ in range(NH)]
        t_ubf = [Tok() for _ in range(NH)]
        t_yb = [Tok() for _ in range(NH)]
        ps = [Sc.ps(name="bps") for _ in range(4)]
        t_ps = [Tok() for _ in range(4)]
        C.op(dve, lambda: nc.vector.memset(lin[:, 0:3], 0.0), writes=[t_lin])
        for c in range(4):
            wbuf = c % 2
            src = w_in_d[l].rearrange("(c p) e -> p c e", p=128)
            C.dma(pool, wl[wbuf][:, :, 0:128], src[:, :, 1536 + c * 128:1536 + (c + 1) * 128], writes=[t_wl[wbuf]])
            C.dma(pool, wl[wbuf][:, :, 128:256], src[:, :, 2048 + c * 128:2048 + (c + 1) * 128], writes=[t_wl[wbuf]])
            C.dma(pool, wab[wbuf][:, 0:128], wa_d[l, c], writes=[t_wab[wbuf]])
            C.dma(pool, wab[wbuf][:, 128:256], wx_d[l, c], writes=[t_wab[wbuf]])
            for i in range(NT):
                tsl = slice(i * TT, (i + 1) * TT)
                k = i % 4
                C.mm(ps[k][:], [(wl[wbuf][:, cc, 0:128], hb[:, cc, tsl]) for cc in range(8)],
                     reads=[t_wl[wbuf], hb_t[i]], writes=[t_ps[k]])
                C.op(act, lambda: nc.scalar.activation(out=lin[:, 3 + i * TT:3 + (i + 1) * TT], in_=ps[k][:], func=AF.Copy),
                     reads=[t_ps[k]], writes=[t_lin])
            cw = lambda k: vcol(l, 56 + c * 4 + k)
            cc_ap = cchan[:, l * 4 + c:l * 4 + c + 1]
            for h in range(NH):
                hs = slice(h * HS, (h + 1) * HS)
                C.op(dve, lambda: nc.vector.tensor_scalar(out=ub[:, hs], in0=lin[:, h * HS:h * HS + HS], scalar1=cw(0), scalar2=vcol(l, 72 + c),
                                                          op0=ALU.mult, op1=ALU.add), reads=[t_lin], writes=[t_ub[h]])
                for k in range(1, 4):
                    C.op(dve, lambda: nc.vector.scalar_tensor_tensor(out=ub[:, hs], in0=lin[:, h * HS + k:h * HS + k + HS], scalar=cw(k),
                                                                     in1=ub[:, hs], op0=ALU.mult, op1=ALU.add),
                         reads=[t_lin, t_ub[h]], writes=[t_ub[h]])
                C.op(act, lambda: nc.scalar.activation(out=ubf[:, hs], in_=ub[:, hs], func=AF.Copy), reads=[t_ub[h]], writes=[t_ubf[h]])
            for h in range(NH):
                for i in range(h * (NT // NH), (h + 1) * (NT // NH)):
                    tsl = slice(i * TT, (i + 1) * TT)
                    k = i % 2
                    C.mm(ps[k][:], [(wab[wbuf][:, 0:128], ubf[:, tsl])], reads=[t_wab[wbuf], t_ubf[h]], writes=[t_ps[k]])
                    C.op(act, lambda: nc.scalar.activation(out=rb_[:, tsl], in_=ps[k][:], func=AF.Sigmoid, bias=vcol(l, 76 + c)),
                         reads=[t_ps[k]], writes=[t_rb[h]])
                    C.mm(ps[2 + k][:], [(wab[wbuf][:, 128:256], ubf[:, tsl])], reads=[t_wab[wbuf], t_ubf[h]], writes=[t_ps[2 + k]])
                    C.op(act, lambda: nc.scalar.activation(out=ib[:, tsl], in_=ps[2 + k][:], func=AF.Sigmoid, bias=vcol(l, 80 + c)),
                         reads=[t_ps[2 + k]], writes=[t_ib[h]])
            for h in range(NH):
                hs = slice(h * HS, (h + 1) * HS)
                C.op(act, lambda: nc.scalar.activation(out=rb_[:, hs], in_=rb_[:, hs], func=AF.Exp, scale=cc_ap),
                     reads=[t_rb[h]], writes=[t_rb[h]])
            for h in range(NH):
                hs = slice(h * HS, (h + 1) * HS)
                C.op(dve, lambda: nc.vector.tensor_tensor(out=ib[:, hs], in0=ib[:, hs], in1=ub[:, hs], op=ALU.mult),
                     reads=[t_ib[h], t_ub[h]], writes=[t_ib[h]])
                C.op(dve, lambda: nc.vector.tensor_tensor(out=ub[:, hs], in0=rb_[:, hs], in1=rb_[:, hs], op=ALU.mult),
                     reads=[t_rb[h], t_ib[h], t_ubf[h]], writes=[t_ub[h]])
                C.op(dve, lambda: nc.vector.tensor_scalar(out=ub[:, hs], in0=ub[:, hs], scalar1=-1.0, scalar2=1.0, op0=ALU.mult, op1=ALU.add),
                     reads=[t_ub[h]], writes=[t_ub[h]])
            for h in range(NH):
                hs = slice(h * HS, (h + 1) * HS)
                C.op(act, lambda: nc.scalar.activation(out=ub[:, hs], in_=ub[:, hs], func=AF.Sqrt), reads=[t_ub[h]], writes=[t_ub[h]])
            for h in range(NH):
                hs = slice(h * HS, (h + 1) * HS)
                C.op(dve, lambda: nc.vector.tensor_tensor(out=ib[:, hs], in0=ib[:, hs], in1=ub[:, hs], op=ALU.mult),
                     reads=[t_ib[h], t_ub[h]], writes=[t_ib[h]])
                init = 0.0 if h == 0 else ub[:, h * HS - 1:h * HS]
                rd = [t_rb[h], t_ib[h], t_ub[h]] + ([t_ub[h - 1]] if h > 0 else [])
                C.op(dve, lambda: nc.vector.tensor_tensor_scan(out=ub[:, hs], data0=rb_[:, hs], data1=ib[:, hs], initial=init,
                                                               op0=ALU.mult, op1=ALU.add), reads=rd, writes=[t_ub[h]])
            for h in range(NH):
                hs = slice(h * HS, (h + 1) * HS)
                for i in range(h * (NT // NH), (h + 1) * (NT // NH)):
                    tsl = slice(i * TT, (i + 1) * TT)
                    k = i % 4
                    C.mm(ps[k][:], [(wl[wbuf][:, cc, 128:256], hb[:, cc, tsl]) for cc in range(8)],
                         reads=[t_wl[wbuf], hb_t[i]], writes=[t_ps[k]])
                    C.op(act, lambda: nc.scalar.activation(out=rb_[:, tsl], in_=ps[k][:], func=AF.Gelu_apprx_tanh),
                         reads=[t_ps[k], t_ub[h]], writes=[t_rb[h]])
                C.op(dve, lambda: nc.vector.tensor_tensor(out=yb[:, hs], in0=rb_[:, hs], in1=ub[:, hs], op=ALU.mult),
                     reads=[t_rb[h], t_ub[h]], writes=[t_yb[h]])
                C.dma(sp, ylru_d[c * 128:(c + 1) * 128, hs], yb[:, hs], reads=[t_yb[h]])
        C.barrier()
        Sc.close()

    def phaseC(l, hb, hb_t):
        Sc = Scope(C)
        wl = [Sc.sb([128, 8, 384], BF16, "wc") for _ in range(2)]
        t_wl = [Tok(), Tok()]
        cb = [Sc.sb([128, S], F32, "ccb") for _ in range(2)]
        ph = [Sc.sb([128, 2 + S], F32, "cph") for _ in range(2)]
        vb = Sc.sb([128, S], F32, "cvb")
        yb = Sc.sb([128, S], BF16, "cyb")
        t_cb, t_ph = [Tok(), Tok()], [Tok(), Tok()]
        t_vb, t_yb = Tok(), Tok()
        ps = [Sc.ps(name="cps") for _ in range(4)]
        t_ps = [Tok() for _ in range(4)]
        for b in range(2):
            C.op(dve, lambda: nc.vector.memset(ph[b][:, 0:2], 0.0), writes=[t_ph[b]])
        src = w_in_d[l].rearrange("(c p) e -> p c e", p=128)

        def front(c):
            wbuf = c % 2
            for j in range(3):
                C.dma(pool, wl[wbuf][:, :, j * 128:(j + 1) * 128], src[:, :, 2560 + j * 512 + c * 128:2560 + j * 512 + (c + 1) * 128],
                      writes=[t_wl[wbuf]])
            for i in range(NT):
                tsl = slice(i * TT, (i + 1) * TT)
                k = i % 2
                C.mm(ps[k][:], [(wl[wbuf][:, cc, 128:256], hb[:, cc, tsl]) for cc in range(8)],
                     reads=[t_wl[wbuf], hb_t[i]], writes=[t_ps[k]])
                C.op(act, lambda: nc.scalar.activation(out=cb[wbuf][:, tsl], in_=ps[k][:], func=AF.Copy), reads=[t_ps[k]], writes=[t_cb[wbuf]])
                C.mm(ps[2 + k][:], [(wl[wbuf][:, cc, 256:384], hb[:, cc, tsl]) for cc in range(8)],
                     reads=[t_wl[wbuf], hb_t[i]], writes=[t_ps[2 + k]])
                C.op(dve, lambda: nc.vector.tensor_tensor(out=ph[wbuf][:, 2 + i * TT:2 + (i + 1) * TT], in0=ps[2 + k][:], in1=cb[wbuf][:, tsl],
                                                          op=ALU.mult), reads=[t_ps[2 + k], t_cb[wbuf]], writes=[t_ph[wbuf]])

        def back(c):
            wbuf = c % 2
            cw = lambda k: vcol(l, 88 + c * 3 + k)
            C.op(dve, lambda: nc.vector.tensor_scalar(out=vb[:], in0=ph[wbuf][:, 0:S], scalar1=cw(0), scalar2=None, op0=ALU.mult),
                 reads=[t_ph[wbuf], t_yb], writes=[t_vb])
            for k in range(1, 3):
                C.op(dve, lambda: nc.vector.scalar_tensor_tensor(out=vb[:], in0=ph[wbuf][:, k:k + S], scalar=cw(k), in1=vb[:],
                                                                 op0=ALU.mult, op1=ALU.add), reads=[t_ph[wbuf], t_vb], writes=[t_vb])
            for i in range(NT):
                tsl = slice(i * TT, (i + 1) * TT)
                k = i % 2
                C.mm(ps[k][:], [(wl[wbuf][:, cc, 0:128], hb[:, cc, tsl]) for cc in range(8)],
                     reads=[t_wl[wbuf], hb_t[i]], writes=[t_ps[k]])
                C.op(dve, lambda: nc.vector.tensor_tensor(out=yb[:, tsl], in0=ps[k][:], in1=vb[:, tsl], op=ALU.mult),
                     reads=[t_ps[k], t_vb], writes=[t_yb])
            C.dma(sp, ysc_d[c * 128:(c + 1) * 128, :], yb[:], reads=[t_yb])

        front(0)
        for c in range(4):
            if c + 1 < 4:
                front(c + 1)
            back(c)
        C.barrier()
        Sc.close()

    def phaseD1(l, hb, hb_t):
        Sc = Scope(C)
        wg = Sc.sb([128, 8, 3072], BF16, "wgate")
        wbr = Sc.sb([128, 12, D], BF16, "wbr")
        t_wgs = [Tok() for _ in range(6)]
        t_wbrs = [Tok() for _ in range(3)]
        src = w_in_d[l].rearrange("(c p) e -> p c e", p=128)

        def ld_wg(j):
            C.dma(pool, wg[:, :, j * 512:(j + 1) * 512], src[:, :, 4096 + j * 512:4096 + (j + 1) * 512], writes=[t_wgs[j]])

        def ld_wbr(br):
            C.dma(pool, wbr[:, br * 4:(br + 1) * 4, :], wbr_d[l, br].rearrange("(c p) d -> p c d", p=128), writes=[t_wbrs[br]])
        for br in range(3):
            ld_wg(2 * br)
            ld_wbr(br)
        for br in range(3):
            ld_wg(2 * br + 1)
        yt = [Sc.sb([128, 12, TT], BF16, "yt") for _ in range(2)]
        t_yt = [Tok(), Tok()]
        mt = [Sc.sb([128, 8, TT], BF16, "mt")] * 2
        _tm = Tok()
        t_mt = [_tm, _tm]
        sg = [Sc.sb([128, TT], F32, "sg") for _ in range(3)]
        t_sg = [Tok() for _ in range(3)]
        acc = [Sc.sb([128, TT], F32, "macc") for _ in range(2)]
        t_acc = [Tok(), Tok()]
        tmp = [Sc.sb([128, TT], F32, "mtmp") for _ in range(2)]
        t_tmp = [Tok(), Tok()]
        gps = [Sc.ps(name="gps") for _ in range(3)]
        pps = [Sc.ps(name="pps") for _ in range(3)]
        t_g = [Tok() for _ in range(3)]
        t_p = [Tok() for _ in range(3)]
        ysrc = [ysb_d, ylru_d, ysc_d]
        def load_y(i):
            for br in range(3):
                C.dma(sp, yt[i % 2][:, br * 4:(br + 1) * 4, :],
                      ysrc[br].rearrange("(c p) t -> p c t", p=128)[:, :, i * TT:(i + 1) * TT], writes=[t_yt[i % 2]])
        load_y(0)
        for i in range(NT):
            tsl = slice(i * TT, (i + 1) * TT)
            b = i % 2
            if i + 1 < NT:
                load_y(i + 1)
            for dc in range(8):
                a = dc % 2
                for br in range(3):
                    col = br * 1024 + dc * 128
                    C.mm(gps[br][:], [(wg[:, cc, col:col + 128], hb[:, cc, tsl]) for cc in range(8)],
                         reads=[t_wgs[col // 512], hb_t[i]], writes=[t_g[br]])
                    C.op(act, lambda: nc.scalar.activation(out=sg[br][:], in_=gps[br][:], func=AF.Sigmoid, bias=vcol(l, 32 + br * 8 + dc)),
                         reads=[t_g[br]], writes=[t_sg[br]])
                    C.mm(pps[br][:], [(wbr[:, br * 4 + wc, dc * 128:(dc + 1) * 128], yt[b][:, br * 4 + wc, :]) for wc in range(4)],
                         reads=[t_wbrs[br], t_yt[b]], writes=[t_p[br]])
                C.op(dve, lambda: nc.vector.tensor_tensor(out=acc[a][:], in0=pps[0][:], in1=sg[0][:], op=ALU.mult),
                     reads=[t_p[0], t_sg[0]], writes=[t_acc[a]])
                C.op(dve, lambda: nc.vector.tensor_tensor(out=tmp[0][:], in0=pps[1][:], in1=sg[1][:], op=ALU.mult),
                     reads=[t_p[1], t_sg[1]], writes=[t_tmp[0]])
                C.op(dve, lambda: nc.vector.tensor_tensor(out=tmp[1][:], in0=pps[2][:], in1=sg[2][:], op=ALU.mult),
                     reads=[t_p[2], t_sg[2]], writes=[t_tmp[1]])
                C.op(pool, lambda: nc.gpsimd.tensor_tensor(out=acc[a][:], in0=acc[a][:], in1=tmp[0][:], op=ALU.add),
                     reads=[t_tmp[0], t_acc[a]], writes=[t_acc[a]])
                C.op(pool, lambda: nc.gpsimd.tensor_tensor(out=mt[b][:, dc, :], in0=acc[a][:], in1=tmp[1][:], op=ALU.add),
                     reads=[t_tmp[1], t_acc[a]], writes=[t_mt[b]])
            C.dma(sp, mT_d[i], mt[b][:], reads=[t_mt[b]])
        C.barrier()
        Sc.close()

    def phaseD2(l):
        Sc = Scope(C)
        L = ln_alloc(Sc, 2)
        lps = [Sc.ps(name="lps") for _ in range(2)]
        t_lps = [Tok(), Tok()]

        def lg_copy(i):
            C.op(dve, lambda: nc.vector.tensor_copy(out=lg[:, i * 4:(i + 1) * 4, :],
                                                    in_=lps[i % 2][:, 0:128].rearrange("p (a b) -> p a b", b=32)[:, :, 0:20]),
                 reads=[t_lps[i % 2]], writes=[t_lg])
        C.dma(sp, wr_sb[:], wr_d[l].rearrange("(c p) e -> p c e", p=128), writes=[t_wr])
        wo = Sc.sb([128, 8, D], BF16, "wo")
        t_wo = Tok()
        C.dma(pool, wo[:], wout_d[l].rearrange("(c p) e -> p c e", p=128), writes=[t_wo])
        NX = 4
        mt = [Sc.sb([128, 8, TT], BF16, "mt2") for _ in range(3)]
        t_mt = [Tok() for _ in range(3)]
        xs = [Sc.sb([128, 8, TT], F32, "xs2") for _ in range(NX)]
        t_xs = [Tok() for _ in range(NX)]
        ops_ = [Sc.ps(name="ops") for _ in range(2)]
        t_o = [Tok(), Tok()]

        def T0(i):
            C.dma(sp, mt[i % 3][:], mT_d[i], writes=[t_mt[i % 3]])
            C.dma(sp, xs[i % NX][:], h32_d[i], writes=[t_xs[i % NX]])

        def T1(i):
            b = i % 3
            x3 = i % NX
            for ec in range(8):
                k = ec % 2
                C.mm(ops_[k][:], [(wo[:, dc, ec * 128:(ec + 1) * 128], mt[b][:, dc, :]) for dc in range(8)],
                     reads=[t_wo, t_mt[b]], writes=[t_o[k]])
                C.op(dve, lambda: nc.vector.scalar_tensor_tensor(out=xs[x3][:, ec, :], in0=xs[x3][:, ec, :], scalar=ALPHA, in1=ops_[k][:],
                                                                 op0=ALU.mult, op1=ALU.add), reads=[t_o[k], t_xs[x3]], writes=[t_xs[x3]])
            ln_s1(L, i, xs[x3], t_xs[x3])

        def T2(i):
            ln_s2(L, i)

        def T3(i):
            ln_s3(L, i, xs[i % NX], t_xs[i % NX], vcol(l, 0, 8), vcol(l, 8, 8), i, False,
                  router=(wr_sb, t_wr, lps, t_lps, lg, t_lg, lg_copy))
        pipeline(NT, [T0, T1, T2, T3], reverse=True)
        lg_copy(NT - 1)
        C.barrier()
        Sc.close()

    def phaseE(l, final):
        Sm = Scope(C)
        cT = Sm.sb([16, S], BF16, "cT")
        t_cT = Tok()
        ST = 2048
        oacc = Sm.sb([128, 8, ST], F32, "oacc")
        Sr = Scope(C)
        lps = [Sr.ps(name="lps") for _ in range(2)]
        t_lps = [Tok(), Tok()]
        NB = 32
        t_r = Tok()

        def rt(shape, name):
            return Sr.sb(shape, F32, name)
        glb = rt([128, NB, 4], "glb")
        gmx = rt([128, NB], "gmx")
        ohg = rt([128, NB, 4], "ohg")
        eg = rt([128, NB, 4], "eg")
        gs = rt([128, NB], "gs")
        psel = rt([128, NB], "psel")
        t44 = rt([128, NB, 4, 4], "t44")
        ein = rt([128, NB, 4], "ein")
        sc = rt([128, NB, 4], "sc")
        m1 = rt([128, NB], "m1")
        oh1 = rt([128, NB, 4], "oh1")
        oh2 = rt([128, NB, 4], "oh2")
        l1 = rt([128, NB], "l1")
        l2 = rt([128, NB], "l2")
        w1 = rt([128, NB], "w1")
        w2 = rt([128, NB], "w2")
        cw = rt([128, NB, 4], "cw")
        comb = rt([128, NB, 4, 4], "comb")
        gl = lg[:, :, 0:4]
        el4 = lg[:, :, 4:20].rearrange("p b (g e) -> p b g e", e=4)
        gbias = rbias[:, l * 20:l * 20 + 4]
        ebias = rbias[:, l * 20 + 4:l * 20 + 20].rearrange("p (g e) -> p g e", e=4)

        def bc3(ap2):
            return ap2.unsqueeze(2).to_broadcast([128, NB, 4])

        def V(fn, reads=(t_lg,)):
            C.op(dve, fn, reads=list(reads) + [t_r], writes=[t_r])

        V(lambda: nc.vector.tensor_tensor(out=glb[:], in0=gl, in1=gbias.unsqueeze(1).to_broadcast([128, NB, 4]), op=ALU.add))
        V(lambda: nc.vector.tensor_reduce(out=gmx[:], in_=glb[:], axis=AX.X, op=ALU.max))
        V(lambda: nc.vector.tensor_tensor(out=ohg[:], in0=glb[:], in1=bc3(gmx[:]), op=ALU.is_equal))
        V(lambda: nc.vector.tensor_reduce(out=gmx[:], in_=gl, axis=AX.X, op=ALU.max))
        V(lambda: nc.vector.tensor_tensor(out=eg[:], in0=gl, in1=bc3(gmx[:]), op=ALU.subtract))
        C.op(act, lambda: nc.scalar.activation(out=eg[:], in_=eg[:], func=AF.Exp), reads=[t_r], writes=[t_r])
        V(lambda: nc.vector.tensor_reduce(out=gs[:], in_=eg[:], axis=AX.X, op=ALU.add))
        V(lambda: nc.vector.tensor_tensor(out=eg[:], in0=eg[:], in1=ohg[:], op=ALU.mult))
        V(lambda: nc.vector.tensor_reduce(out=psel[:], in_=eg[:], axis=AX.X, op=ALU.add))
        V(lambda: nc.vector.reciprocal(out=gs[:], in_=gs[:]))
        V(lambda: nc.vector.tensor_tensor(out=psel[:], in0=psel[:], in1=gs[:], op=ALU.mult))
        ohg4 = ohg[:].unsqueeze(3).to_broadcast([128, NB, 4, 4])
        V(lambda: nc.vector.tensor_tensor(out=t44[:], in0=el4, in1=ohg4, op=ALU.mult))
        V(lambda: nc.vector.tensor_reduce(out=ein[:], in_=t44[:].rearrange("p b g e -> p b e g"), axis=AX.X, op=ALU.add))
        V(lambda: nc.vector.tensor_tensor(out=t44[:], in0=ohg4, in1=ebias.unsqueeze(1).to_broadcast([128, NB, 4, 4]), op=ALU.mult))
        V(lambda: nc.vector.tensor_reduce(out=sc[:], in_=t44[:].rearrange("p b g e -> p b e g"), axis=AX.X, op=ALU.add))
        V(lambda: nc.vector.tensor_tensor(out=sc[:], in0=sc[:], in1=ein[:], op=ALU.add))
        V(lambda: nc.vector.tensor_reduce(out=m1[:], in_=sc[:], axis=AX.X, op=ALU.max))
        V(lambda: nc.vector.tensor_tensor(out=oh1[:], in0=sc[:], in1=bc3(m1[:]), op=ALU.is_equal))
        V(lambda: nc.vector.scalar_tensor_tensor(out=sc[:], in0=oh1[:], scalar=-1e30, in1=sc[:], op0=ALU.mult, op1=ALU.add))
        V(lambda: nc.vector.tensor_reduce(out=m1[:], in_=sc[:], axis=AX.X, op=ALU.max))
        V(lambda: nc.vector.tensor_tensor(out=oh2[:], in0=sc[:], in1=bc3(m1[:]), op=ALU.is_equal))
        V(lambda: nc.vector.tensor_tensor(out=sc[:], in0=oh1[:], in1=ein[:], op=ALU.mult))
        V(lambda: nc.vector.tensor_reduce(out=l1[:], in_=sc[:], axis=AX.X, op=ALU.add))
        V(lambda: nc.vector.tensor_tensor(out=sc[:], in0=oh2[:], in1=ein[:], op=ALU.mult))
        V(lambda: nc.vector.tensor_reduce(out=l2[:], in_=sc[:], axis=AX.X, op=ALU.add))
        V(lambda: nc.vector.tensor_tensor(out=l1[:], in0=l1[:], in1=l2[:], op=ALU.subtract))
        C.op(act, lambda: nc.scalar.activation(out=w1[:], in_=l1[:], func=AF.Sigmoid), reads=[t_r], writes=[t_r])
        V(lambda: nc.vector.tensor_tensor(out=w1[:], in0=w1[:], in1=psel[:], op=ALU.mult))
        V(lambda: nc.vector.tensor_tensor(out=w2[:], in0=psel[:], in1=w1[:], op=ALU.subtract))
        V(lambda: nc.vector.tensor_tensor(out=cw[:], in0=oh1[:], in1=bc3(w1[:]), op=ALU.mult))
        V(lambda: nc.vector.tensor_tensor(out=oh2[:], in0=oh2[:], in1=bc3(w2[:]), op=ALU.mult))
        V(lambda: nc.vector.tensor_tensor(out=cw[:], in0=cw[:], in1=oh2[:], op=ALU.add))
        V(lambda: nc.vector.tensor_tensor(out=comb[:], in0=ohg4, in1=cw[:].unsqueeze(2).to_broadcast([128, NB, 4, 4]), op=ALU.mult))
        for i in range(NT):
            b = i % 2
            for tb in range(4):
                blk = i * 4 + tb
                C.mm(lps[b][0:16, tb * 128:(tb + 1) * 128], [(comb[:, blk].rearrange("p g e -> p (g e)"), ident[:])],
                     reads=[t_r], writes=[t_lps[b]])
            C.op(act, lambda: nc.scalar.activation(out=cT[:, i * TT:(i + 1) * TT], in_=lps[b][0:16, :], func=AF.Copy),
                 reads=[t_lps[b]], writes=[t_cT])
        if debug:
            C.dma(sp, dbg_comb[:, :, :], comb[:].rearrange("p b g e -> p b (g e)"), reads=[t_r])
        C.barrier()
        Sr.close()

        t_hbs = [Tok() for _ in range(4)]
        t_oacc = [[Tok() for _ in range(8)] for _ in range(4)]
        for st in range(2):
            Se = Scope(C)
            hbs = Se.sb([128, 8, ST], BF16, "hbs")
            for t in range(4):
                g0 = st * ST + t * TT
                C.dma(sp, hbs[:, :, t * TT:(t + 1) * TT], hbf_d[st * 4 + t], writes=[t_hbs[t]])
            wgu = [Se.sb([128, 16, 512], BF16, "wgu") for _ in range(2)]
            wdn = [Se.sb([128, 4, D], BF16, "wdn") for _ in range(2)]
            t_wgu = [Tok(), Tok()]
            t_wdn = [Tok(), Tok()]
            cbs = [Se.sb([128, TT], F32, "cbs") for _ in range(2)]
            t_cbs = [Tok(), Tok()]
            sgb = [Se.sb([128, TT], F32, "sgb") for _ in range(2)]
            t_sgb = [Tok(), Tok()]
            t1b = [Se.sb([128, TT], F32, "t1b") for _ in range(2)]
            t_t1b = [Tok(), Tok()]
            hc = [Se.sb([128, 4, TT], BF16, "hc") for _ in range(2)]
            t_hc = [Tok(), Tok()]
            gp = [Se.ps(name="gp") for _ in range(2)]
            up = [Se.ps(name="up") for _ in range(2)]
            dp = [Se.ps(name="dp") for _ in range(2)]
            cbp = Se.ps(name="cbp")
            t_gp = [Tok(), Tok()]
            t_up = [Tok(), Tok()]
            t_dp = [Tok(), Tok()]
            t_cbp = Tok()

            def load_expert(e):
                wbuf = e % 2
                C.dma(pool, wgu[wbuf][:, 0:8, :], wg_d[l, e].rearrange("(c p) f -> p c f", p=128), writes=[t_wgu[wbuf]])
                C.dma(pool, wgu[wbuf][:, 8:16, :], wu_d[l, e].rearrange("(c p) f -> p c f", p=128), writes=[t_wgu[wbuf]])
                C.dma(pool, wdn[wbuf][:], wd_d[l, e].rearrange("(c p) d -> p c d", p=128), writes=[t_wdn[wbuf]])

            load_expert(0)

            def GU(n):
                e, t = n // 4, n % 4
                wbuf = e % 2
                if t == 1 and e + 1 < 16:
                    load_expert(e + 1)
                g0 = st * ST + t * TT
                tl = slice(t * TT, (t + 1) * TT)
                hb_i = n % 2
                C.mm(cbp[:], [(sel[:, e * 128:(e + 1) * 128], cT[:, g0:g0 + TT])], reads=[t_cT], writes=[t_cbp])
                C.op(act, lambda: nc.scalar.activation(out=cbs[hb_i][:], in_=cbp[:], func=AF.Copy), reads=[t_cbp], writes=[t_cbs[hb_i]])
                for fc in range(4):
                    k = fc % 2
                    C.mm(gp[k][:], [(wgu[wbuf][:, c, fc * 128:(fc + 1) * 128], hbs[:, c, tl]) for c in range(8)],
                         reads=[t_wgu[wbuf], t_hbs[t]], writes=[t_gp[k]])
                    C.mm(up[k][:], [(wgu[wbuf][:, 8 + c, fc * 128:(fc + 1) * 128], hbs[:, c, tl]) for c in range(8)],
                         reads=[t_wgu[wbuf], t_hbs[t]], writes=[t_up[k]])
                    C.op(act, lambda: nc.scalar.activation(out=sgb[k][:], in_=gp[k][:], func=AF.Silu), reads=[t_gp[k]], writes=[t_sgb[k]])
                    C.op(dve, lambda: nc.vector.tensor_tensor(out=t1b[k][:], in0=up[k][:], in1=sgb[k][:], op=ALU.mult),
                         reads=[t_up[k], t_sgb[k]], writes=[t_t1b[k]])
                    C.op(pool, lambda: nc.gpsimd.tensor_tensor(out=hc[hb_i][:, fc, :], in0=t1b[k][:], in1=cbs[hb_i][:], op=ALU.mult),
                         reads=[t_t1b[k], t_cbs[hb_i]], writes=[t_hc[hb_i]])

            def DN(n):
                e, t = n // 4, n % 4
                wbuf = e % 2
                tl = slice(t * TT, (t + 1) * TT)
                hb_i = n % 2
                for dc in range(8):
                    k = dc % 2
                    C.mm(dp[k][:], [(wdn[wbuf][:, fc, dc * 128:(dc + 1) * 128], hc[hb_i][:, fc, :]) for fc in range(4)],
                         reads=[t_wdn[wbuf], t_hc[hb_i]], writes=[t_dp[k]])
                    if e == 0:
                        C.op(act, lambda: nc.scalar.activation(out=oacc[:, dc, tl], in_=dp[k][:], func=AF.Copy),
                             reads=[t_dp[k]], writes=[t_oacc[t][dc]])
                    else:
                        C.op(dve, lambda: nc.vector.tensor_tensor(out=oacc[:, dc, tl], in0=oacc[:, dc, tl], in1=dp[k][:], op=ALU.add),
                             reads=[t_dp[k], t_oacc[t][dc]], writes=[t_oacc[t][dc]])
            pipeline(64, [GU, DN])
            C.barrier()
            Se.close()
            Sl = Scope(C)
            L = ln_alloc(Sl, 2, final)
            xs = [Sl.sb([128, 8, TT], F32, "xs3") for _ in range(3)]
            t_xs = [Tok() for _ in range(3)]

            def T0(t):
                C.dma(sp, xs[t % 3][:], h32_d[st * 4 + t], writes=[t_xs[t % 3]])

            def T1(t):
                x3 = t % 3
                tl = slice(t * TT, (t + 1) * TT)
                C.op(dve, lambda: nc.vector.scalar_tensor_tensor(out=xs[x3][:], in0=xs[x3][:], scalar=ALPHA, in1=oacc[:, :, tl],
                                                                 op0=ALU.mult, op1=ALU.add), reads=[t_xs[x3]] + t_oacc[t], writes=[t_xs[x3]])
                ln_s1(L, t, xs[x3], t_xs[x3])

            def T2(t):
                ln_s2(L, t)

            def T3(t):
                ln_s3(L, t, xs[t % 3], t_xs[t % 3], vcol(l, 16, 8), vcol(l, 24, 8), st * 4 + t, final)
            pipeline(4, [T0, T1, T2, T3], reverse=True)
            C.barrier()
            Sl.close()
        Sm.close()

    dbg_comb = None
    if debug:
        dbg_comb = nc.dram_tensor("dbg_comb", [128, 32, 16], F32, kind="ExternalOutput").ap()

    def run():
        phase0()
        if stop == "p0":
            return
        for l in range(nlayers):
            Sh = Scope(C)
            hb, hb_t = load_hbf(Sh)
            phaseA(l, hb, hb_t)
            if stop == "A":
                Sh.close()
                return
            phaseB(l, hb, hb_t)
            if stop == "B":
                Sh.close()
                return
            phaseC(l, hb, hb_t)
            if stop == "C":
                Sh.close()
                return
            phaseD1(l, hb, hb_t)
            Sh.close()
            if stop == "D1":
                return
            phaseD2(l)
            if stop == "D2":
                return
            phaseE(l, final=(l == nlayers - 1 and stop is None))
            if stop == "E":
                return

    run()
    C.barrier()
    G.close()
    C.es.close()
    return nc


def _fmaj(v, n):
    return np.ascontiguousarray(np.asarray(v, np.float32).reshape(n, 128).T)


def prepare_inputs(inputs):
    f32 = np.float32
    g = {k: np.asarray(v) for k, v in inputs.items()}
    vecs = np.zeros((DEPTH, 128, NV), f32)
    rb = np.zeros((DEPTH, 128, 20), f32)
    wa_bd = np.zeros((DEPTH, 4, 128, 128), f32)
    wx_bd = np.zeros((DEPTH, 4, 128, 128), f32)
    for l in range(DEPTH):
        vecs[l, :, 0:8] = _fmaj(g["ln1_g"][l], 8)
        vecs[l, :, 8:16] = _fmaj(g["ln1_b"][l], 8)
        vecs[l, :, 16:24] = _fmaj(g["ln2_g"][l], 8)
        vecs[l, :, 24:32] = _fmaj(g["ln2_b"][l], 8)
        for br in range(3):
            vecs[l, :, 32 + br * 8:40 + br * 8] = _fmaj(g["gate_b"][l, br], 8)
        for k in range(4):
            vecs[l, :, 56 + k:72:4] = _fmaj(g["lru_conv_w"][l, k], 4)
        vecs[l, :, 72:76] = _fmaj(g["lru_conv_b"][l], 4)
        vecs[l, :, 76:80] = _fmaj(g["lru_ba"][l], 4)
        vecs[l, :, 80:84] = _fmaj(g["lru_bx"][l], 4)
        vecs[l, :, 84:88] = _fmaj(g["lru_lambda"][l], 4)
        for k in range(3):
            vecs[l, :, 88 + k:100:3] = _fmaj(g["sc_conv_w"][l, k], 4)
        rb[l, :, 0:4] = g["group_bias"][l][None, :]
        rb[l, :, 4:20] = g["expert_bias"][l][None, :]
        for c in range(4):
            for h in range(2):
                wa_bd[l, c, h * 64:(h + 1) * 64, h * 64:(h + 1) * 64] = g["lru_wa"][l, 2 * c + h]
                wx_bd[l, c, h * 64:(h + 1) * 64, h * 64:(h + 1) * 64] = g["lru_wx"][l, 2 * c + h]
    lnin = np.concatenate([_fmaj(g["ln_in_g"], 8), _fmaj(g["ln_in_b"], 8)], axis=1)
    w_branch = np.ascontiguousarray(np.stack([g["w_branch_sb"], g["w_branch_lru"], g["w_branch_sc"]], axis=1).astype(f32))
    w_router = np.ascontiguousarray(np.concatenate([g["w_group"], g["w_expert_router"]], axis=2).astype(f32))
    ident = np.eye(128, dtype=f32)
    jj, ss = np.meshgrid(np.arange(128), np.arange(128), indexing="ij")
    negtri = np.where(jj >= ss, -1.0, 0.0).astype(f32)
    sidx = np.arange(128)[:, None]
    tidx = np.arange(512)[None, :]
    masks = np.concatenate([(tidx > sidx + 128 * j).astype(f32) for j in (0, 0, 1, 1, 2, 2, 3, 3)], axis=1)
    sel = np.zeros((16, 16, 128), f32)
    for e in range(16):
        sel[e, e, :] = 1.0
    sel = sel.reshape(16, 16 * 128)
    shared = dict(w_in=np.ascontiguousarray(g["w_in"].astype(f32)), wa_bd=wa_bd, wx_bd=wx_bd, w_branch=w_branch,
                  w_out=np.ascontiguousarray(g["w_out"].astype(f32)), w_router=w_router,
                  w_gate=np.ascontiguousarray(g["w_gate"].astype(f32)), w_up=np.ascontiguousarray(g["w_up"].astype(f32)),
                  w_down=np.ascontiguousarray(g["w_down"].astype(f32)), vecs=vecs, lnin=np.ascontiguousarray(lnin),
                  rbias=rb, ident=ident, negtri=negtri, masks=np.ascontiguousarray(masks), sel=sel)
    x = np.asarray(g["x"], f32)
    in_maps = []
    for c in range(NCORES):
        m = dict(shared)
        m["x"] = np.ascontiguousarray(x[c])
        in_maps.append(m)
    return in_maps


def kernel(**inputs):
    in_maps = prepare_inputs(inputs)
    nc = build()
    res = run_bass_kernel_spmd(nc, in_maps, core_ids=list(range(NCORES)))
    return np.stack([np.asarray(res.results[c]["out"], np.float32) for c in range(NCORES)], axis=0)
```
